# Optimizing a Trainium2 kernel written in Bass

```python
import jax, jax.numpy as jnp
from jax import lax
import numpy as np

D_MODEL = 1024
BATCH = 8
SEQ = 2048
DEPTH = 4

N_MIXERS = 2
MOBA_HEADS = 16
MOBA_HEAD_DIM = D_MODEL // MOBA_HEADS
MOBA_BLOCK = 256
MOBA_TOPK = 3
MOBA_QCHUNK = 128
GMLP_EXPAND = 6
GMLP_DV = GMLP_EXPAND * D_MODEL // 2
GMLP_GROUPS = 8
GMLP_GROUP_DIM = GMLP_DV // GMLP_GROUPS
GMLP_CHUNK = 128
MOE_GROUPS = 8
MOE_EXPERTS_PER_GROUP = 8
MOE_EXPERTS = MOE_GROUPS * MOE_EXPERTS_PER_GROUP
MOE_TOPK = 2
MOE_D_EXPERT = D_MODEL // 4
MOE_ROW_BLOCK = 128
LN_EPS = 1e-5
DEEPNORM_ALPHA = (2.0 * DEPTH) ** 0.25
DEEPNORM_BETA = (8.0 * DEPTH) ** -0.25
N_MOBA_LAYERS = (DEPTH + 1) // 2
N_GMLP_LAYERS = DEPTH // 2

kernel_name = "hybrid_moba_gmlp_hmoe_deepnorm"


def layer_norm(x, g, b):
    xf = x.astype(jnp.float32)
    mu = jnp.mean(xf, axis=-1, keepdims=True)
    var = jnp.mean(jnp.square(xf - mu), axis=-1, keepdims=True)
    y = (xf - mu) * lax.rsqrt(var + LN_EPS)
    return (y * g + b).astype(x.dtype)


def moba_attention(x, w_qkv, w_o):
    B, S, D = x.shape
    H, DH, KB, QC = MOBA_HEADS, MOBA_HEAD_DIM, MOBA_BLOCK, MOBA_QCHUNK
    nb = -(-S // KB)
    nq = S // QC
    k_sel = min(MOBA_TOPK, nb)
    scale = DH ** -0.5

    qkv = (x @ w_qkv).reshape(B, S, 3, H, DH)
    q = qkv[:, :, 0].transpose(0, 2, 1, 3)
    k = qkv[:, :, 1].transpose(0, 2, 1, 3)
    v = qkv[:, :, 2].transpose(0, 2, 1, 3)
    pad = nb * KB - S
    k_blocks = jnp.pad(k, ((0, 0), (0, 0), (0, pad), (0, 0))).reshape(B, H, nb, KB, DH)
    v_blocks = jnp.pad(v, ((0, 0), (0, 0), (0, pad), (0, 0))).reshape(B, H, nb, KB, DH)
    k_mean = jnp.mean(k_blocks.astype(jnp.float32), axis=3)

    q_chunks = q.reshape(B, H, nq, QC, DH).transpose(0, 2, 1, 3, 4).reshape(B * nq, H, QC, DH)
    b_ids = jnp.repeat(jnp.arange(B), nq)
    n_ids = jnp.tile(jnp.arange(nq), B)
    head_ids = jnp.arange(H)[:, None, None]

    def step(args):
        qc, b, n = args
        kb, vb, km = k_blocks[b], v_blocks[b], k_mean[b]
        cur = (n * QC) // KB
        blk_scores = jnp.einsum('hqd,hjd->hqj', qc.astype(jnp.float32), km)
        past = jnp.arange(nb) < cur
        blk_scores = jnp.where(past[None, None, :], blk_scores, -jnp.inf)
        _, sel = lax.top_k(blk_scores, k_sel)
        sel_valid = sel < cur
        kg = kb[head_ids, sel]
        vg = vb[head_ids, sel]
        s_sel = jnp.einsum('hqd,hqjkd->hqjk', qc, kg,
                           preferred_element_type=jnp.float32) * scale
        s_sel = jnp.where(sel_valid[..., None], s_sel, -jnp.inf).reshape(H, QC, k_sel * KB)
        k_own = lax.dynamic_slice_in_dim(kb, cur, 1, axis=1)[:, 0]
        v_own = lax.dynamic_slice_in_dim(vb, cur, 1, axis=1)[:, 0]
        s_own = jnp.einsum('hqd,hkd->hqk', qc, k_own,
                           preferred_element_type=jnp.float32) * scale
        q_pos = n * QC + jnp.arange(QC)
        k_pos = cur * KB + jnp.arange(KB)
        s_own = jnp.where((k_pos[None, :] <= q_pos[:, None])[None], s_own, -jnp.inf)
        p = jax.nn.softmax(jnp.concatenate([s_sel, s_own], axis=-1), axis=-1)
        p_sel = p[..., :k_sel * KB].reshape(H, QC, k_sel, KB).astype(vg.dtype)
        p_own = p[..., k_sel * KB:].astype(v_own.dtype)
        out = (jnp.einsum('hqjk,hqjkd->hqd', p_sel, vg)
               + jnp.einsum('hqk,hkd->hqd', p_own, v_own))
        return out.astype(x.dtype)

    o = lax.map(step, (q_chunks, b_ids, n_ids))
    o = o.reshape(B, nq, H, QC, DH).transpose(0, 1, 3, 2, 4).reshape(B, S, D)
    return o @ w_o


def chunked_gmlp(x, w_in, b_in, ln_v_g, ln_v_b, w_s, b_s, w_out):
    B, S, _ = x.shape
    z = jax.nn.gelu(x @ w_in + b_in, approximate=False)
    u, v = jnp.split(z, 2, axis=-1)
    v = layer_norm(v, ln_v_g, ln_v_b)
    v = v.reshape(B, S // GMLP_CHUNK, GMLP_CHUNK, GMLP_GROUPS, GMLP_GROUP_DIM)
    w_causal = jnp.tril(w_s)
    mixed = jnp.einsum('gts,bcsgk->bctgk', w_causal, v) + b_s.T[:, :, None]
    return (u * mixed.reshape(B, S, GMLP_DV)) @ w_out


def grouped_expert_ffn(xf, expert_id, gates, w1, w3, w2):
    T, D = xf.shape
    E, R = MOE_EXPERTS, MOE_ROW_BLOCK
    A = expert_id.size
    n_blk = -(-A // R) + E
    flat_e = expert_id.reshape(-1)
    order = jnp.argsort(flat_e)
    sorted_e = flat_e[order]
    tok = order // expert_id.shape[1]
    counts = jnp.bincount(flat_e, length=E)
    start = jnp.cumsum(counts) - counts
    padded = ((counts + R - 1) // R) * R
    pad_end = jnp.cumsum(padded)
    pad_start = pad_end - padded
    dest = pad_start[sorted_e] + (jnp.arange(A) - start[sorted_e])
    buf = jnp.zeros((n_blk * R, D), xf.dtype).at[dest].set(xf[tok])
    blk_expert = jnp.clip(jnp.searchsorted(pad_end, jnp.arange(n_blk) * R, side='right'), 0, E - 1)

    def run_block(args):
        rows, e = args
        h = jax.nn.silu(rows @ w1[e]) * (rows @ w3[e])
        return h @ w2[e]

    y_buf = lax.map(run_block, (buf.reshape(n_blk, R, D), blk_expert)).reshape(n_blk * R, D)
    g_sorted = gates.reshape(-1)[order].astype(xf.dtype)
    return jax.ops.segment_sum(y_buf[dest] * g_sorted[:, None], tok, num_segments=T)


def hier_moe(x, w_grp, b_grp, w_rt, b_rt, w1, w3, w2):
    B, S, D = x.shape
    T = B * S
    xf = x.reshape(T, D)
    g_prob = jax.nn.softmax((xf @ w_grp + b_grp).astype(jnp.float32), axis=-1)
    g_p, g_idx = lax.top_k(g_prob, 1)
    e_logits = (xf @ w_rt + b_rt).astype(jnp.float32).reshape(T, MOE_GROUPS, MOE_EXPERTS_PER_GROUP)
    e_logits = jnp.take_along_axis(e_logits, g_idx[:, :, None], axis=1)[:, 0]
    e_top, e_local = lax.top_k(e_logits, MOE_TOPK)
    gates = g_p * jax.nn.softmax(e_top, axis=-1)
    expert_id = g_idx * MOE_EXPERTS_PER_GROUP + e_local
    return grouped_expert_ffn(xf, expert_id, gates, w1, w3, w2).reshape(B, S, D)


def setup_inputs(seed: int = 0) -> dict:
    key = jax.random.key(seed)
    ks = jax.random.split(key, 24)
    D, E, F = D_MODEL, MOE_EXPERTS, MOE_D_EXPERT
    nrm = lambda k, shape, s: jax.random.normal(k, shape, jnp.float32) * s
    return {
        "x": nrm(ks[0], (BATCH, SEQ, D), 1.0),
        "moba_w_qkv": nrm(ks[1], (N_MOBA_LAYERS, D, 3 * D), D ** -0.5),
        "moba_w_o": nrm(ks[2], (N_MOBA_LAYERS, D, D), D ** -0.5 * DEEPNORM_BETA),
        "gmlp_w_in": nrm(ks[3], (N_GMLP_LAYERS, D, 2 * GMLP_DV), D ** -0.5),
        "gmlp_b_in": nrm(ks[4], (N_GMLP_LAYERS, 2 * GMLP_DV), 0.02),
        "gmlp_ln_g": 1.0 + nrm(ks[5], (N_GMLP_LAYERS, GMLP_DV), 0.02),
        "gmlp_ln_b": nrm(ks[6], (N_GMLP_LAYERS, GMLP_DV), 0.02),
        "gmlp_w_s": nrm(ks[7], (N_GMLP_LAYERS, GMLP_GROUPS, GMLP_CHUNK, GMLP_CHUNK), GMLP_CHUNK ** -0.5),
        "gmlp_b_s": 1.0 + nrm(ks[8], (N_GMLP_LAYERS, GMLP_GROUPS, GMLP_CHUNK), 0.02),
        "gmlp_w_out": nrm(ks[9], (N_GMLP_LAYERS, GMLP_DV, D), GMLP_DV ** -0.5 * DEEPNORM_BETA),
        "ln1_g": 1.0 + nrm(ks[10], (DEPTH, D), 0.02),
        "ln1_b": nrm(ks[11], (DEPTH, D), 0.02),
        "ln2_g": 1.0 + nrm(ks[12], (DEPTH, D), 0.02),
        "ln2_b": nrm(ks[13], (DEPTH, D), 0.02),
        "moe_w_grp": nrm(ks[14], (DEPTH, D, MOE_GROUPS), D ** -0.5),
        "moe_b_grp": nrm(ks[15], (DEPTH, MOE_GROUPS), 0.01),
        "moe_w_rt": nrm(ks[16], (DEPTH, D, E), D ** -0.5),
        "moe_b_rt": nrm(ks[17], (DEPTH, E), 0.01),
        "moe_w1": nrm(ks[18], (DEPTH, E, D, F), D ** -0.5),
        "moe_w3": nrm(ks[19], (DEPTH, E, D, F), D ** -0.5),
        "moe_w2": nrm(ks[20], (DEPTH, E, F, D), F ** -0.5 * DEEPNORM_BETA),
    }


def reference(x, moba_w_qkv, moba_w_o, gmlp_w_in, gmlp_b_in, gmlp_ln_g, gmlp_ln_b, gmlp_w_s,
              gmlp_b_s, gmlp_w_out, ln1_g, ln1_b, ln2_g, ln2_b, moe_w_grp, moe_b_grp, moe_w_rt,
              moe_b_rt, moe_w1, moe_w3, moe_w2):
    for i in range(DEPTH):
        j = i // N_MIXERS
        if i % N_MIXERS == 0:
            h = moba_attention(x, moba_w_qkv[j], moba_w_o[j])
        else:
            h = chunked_gmlp(x, gmlp_w_in[j], gmlp_b_in[j], gmlp_ln_g[j], gmlp_ln_b[j],
                             gmlp_w_s[j], gmlp_b_s[j], gmlp_w_out[j])
        x = layer_norm(DEEPNORM_ALPHA * x + h, ln1_g[i], ln1_b[i])
        h = hier_moe(x, moe_w_grp[i], moe_b_grp[i], moe_w_rt[i], moe_b_rt[i],
                     moe_w1[i], moe_w3[i], moe_w2[i])
        x = layer_norm(DEEPNORM_ALPHA * x + h, ln2_g[i], ln2_b[i])
    return x
```

```python
import numpy as np
import ml_dtypes
from contextlib import ExitStack
import concourse.bass as bass
import concourse.mybir as mybir
from concourse.bass_utils import run_bass_kernel_spmd

F32 = mybir.dt.float32
BF16 = mybir.dt.bfloat16
I32 = mybir.dt.int32
AF = mybir.ActivationFunctionType
ALU = mybir.AluOpType
AX = mybir.AxisListType

D = 1024
SEQ = 2048
NT = 16
DC = 8
DEPTH = 4
ALPHA = float(8.0 ** 0.25)
EPS = 1e-5
NEG = -30000.0
CAP = 128
NSLOT = 64 * CAP
DV = 3072
import os
DBG = set(os.environ.get('KDBG', '').split(','))
STOP = int(os.environ.get('KSTOP', '0'))


class Sched:
    R = 16

    def __init__(self, nc, strict_same=True):
        self.nc = nc
        self.strict_same = strict_same
        self.eng = {"pe": nc.tensor, "act": nc.scalar, "dve": nc.vector, "pool": nc.gpsimd, "sp": nc.sync}
        self.sem = {}
        self.cnt = {}
        self.seen = {k: {} for k in self.eng}
        self.stack = ExitStack()
        for k in self.eng:
            self.sem[k] = self.stack.enter_context(nc.semaphore("sem_" + k))
            self.cnt[k] = 0
        self.rings = {}
        for k in ("sp", "pool", "act"):
            self.rings[k] = {"sems": [self.stack.enter_context(nc.semaphore(f"ring_{k}_{i}")) for i in range(self.R)],
                             "n": 0}
        self.lastw = {}
        self.readers = {}

    def _wait(self, qn, tok):
        if tok is None:
            return
        kind, src, idx = tok
        if kind == "c":
            if src == qn and (qn == "pe" or not self.strict_same):
                return
            key = ("c", src)
            val = idx
            sem = self.sem[src]
        else:
            ring = self.rings[src]
            slot = idx % self.R
            key = ("d", src, slot)
            val = 16 * (idx // self.R + 1)
            sem = ring["sems"][slot]
        if self.seen[qn].get(key, 0) >= val:
            return
        self.eng[qn].wait_ge(sem, val)
        self.seen[qn][key] = val

    def _deps(self, reads, writes):
        deps = []
        for r in reads:
            t = self.lastw.get(r)
            if t is not None:
                deps.append(t)
        for w in writes:
            t = self.lastw.get(w)
            if t is not None:
                deps.append(t)
            deps.extend(self.readers.get(w, ()))
        return deps

    def _commit(self, tok, reads, writes):
        for r in reads:
            self.readers.setdefault(r, []).append(tok)
        for w in writes:
            self.lastw[w] = tok
            self.readers[w] = []

    def op(self, qn, fns, reads=(), writes=()):
        for t in self._deps(reads, writes):
            self._wait(qn, t)
        if callable(fns):
            fns = [fns]
        inst = None
        for f in fns:
            inst = f()
        inst.then_inc(self.sem[qn], 1)
        self.cnt[qn] += 1
        tok = ("c", qn, self.cnt[qn])
        self._commit(tok, reads, writes)
        return tok

    def dma(self, qn, fn, reads=(), writes=()):
        ring = self.rings[qn]
        i = ring["n"]
        if i >= self.R:
            self._wait(qn, ("d", qn, i - self.R))
        for t in self._deps(reads, writes):
            self._wait(qn, t)
        inst = fn()
        inst.then_inc(ring["sems"][i % self.R], 16)
        ring["n"] += 1
        tok = ("d", qn, i)
        self._commit(tok, reads, writes)
        return tok

    def acquire(self, qn, keys):
        for t in self._deps((), keys):
            self._wait(qn, t)

    def barrier(self):
        toks = []
        for k in self.eng:
            if self.cnt[k] > 0:
                toks.append(("c", k, self.cnt[k]))
        for k, ring in self.rings.items():
            n = ring["n"]
            for i in range(max(0, n - self.R), n):
                toks.append(("d", k, i))
        for qn in self.eng:
            for t in toks:
                if t[0] == "c" and t[1] == qn:
                    continue
                self._wait(qn, t)

    def finish(self, qn="sp"):
        toks = []
        for k, ring in self.rings.items():
            n = ring["n"]
            for i in range(max(0, n - self.R), n):
                toks.append(("d", k, i))
        for k in self.eng:
            if self.cnt[k] > 0 and k != qn:
                toks.append(("c", k, self.cnt[k]))
        for t in toks:
            self._wait(qn, t)


def host_consts():
    c = {}
    c["c_ident_f"] = np.eye(128, dtype=np.float32)
    k = np.arange(128)
    tri = (k[:, None] <= k[None, :]).astype(np.float32)
    ustr = (k[:, None] < k[None, :]).astype(np.float32)
    c["c_tri"] = tri
    c["c_ustr"] = ustr
    blk = (np.arange(SEQ)[None, :] // 256 == np.arange(8)[:, None]).astype(np.float32)
    c["c_blkind"] = blk
    c["c_slotbase"] = np.tile((np.arange(64) * CAP).astype(np.float32)[None, :], (128, 1))
    return c


class K:
    def __init__(self, stages, debug=False, strict_same=True):
        self.stages = stages
        nc = bass.Bass("TRN2", target_bir_lowering=False)
        self.nc = nc
        self.S = Sched(nc, strict_same=strict_same)
        self.es = ExitStack()
        dt = lambda n, shp, kind="ExternalInput", d=F32: nc.dram_tensor(n, shp, d, kind=kind).ap()
        self.x_in = dt("x", [SEQ, D])
        self.out = dt("out", [SEQ, D], kind="ExternalOutput")
        self.w = {}
        self.w["moba_w_qkv"] = dt("moba_w_qkv", [2, D, 3 * D])
        self.w["moba_w_o"] = dt("moba_w_o", [2, D, D])
        self.w["gmlp_w_in"] = dt("gmlp_w_in", [2, D, 2 * DV])
        self.w["gmlp_b_in"] = dt("gmlp_b_in", [2, 2 * DV])
        self.w["gmlp_ln_g"] = dt("gmlp_ln_g", [2, DV])
        self.w["gmlp_ln_b"] = dt("gmlp_ln_b", [2, DV])
        self.w["gmlp_w_sT"] = dt("gmlp_w_sT", [2, 128, 8, 128])
        self.w["gmlp_b_sT"] = dt("gmlp_b_sT", [2, 128, 8])
        self.w["gmlp_w_out"] = dt("gmlp_w_out", [2, DV, D])
        for n in ("ln1_g", "ln1_b", "ln2_g", "ln2_b"):
            self.w[n] = dt(n, [DEPTH, D])
        self.w["moe_w_r"] = dt("moe_w_r", [DEPTH, D, 72])
        self.w["moe_b_r"] = dt("moe_b_r", [DEPTH, 72])
        self.w["moe_w1"] = dt("moe_w1", [DEPTH, 64, D, 256])
        self.w["moe_w3"] = dt("moe_w3", [DEPTH, 64, D, 256])
        self.w["moe_w2"] = dt("moe_w2", [DEPTH, 64, 256, D])
        self.c = {}
        self.c["ident_f"] = dt("c_ident_f", [128, 128])
        self.c["tri"] = dt("c_tri", [128, 128])
        self.c["ustr"] = dt("c_ustr", [128, 128])
        self.c["blkind"] = dt("c_blkind", [8, SEQ])
        self.c["slotbase"] = dt("c_slotbase", [128, 64])
        self.xg_d = dt("xg_scratch", [NSLOT, D], kind="Internal", d=BF16)
        self.y_d = dt("y_scratch", [NSLOT, D], kind="Internal", d=BF16)

    def sb(self, es, name, shape, dtype):
        self._uid = getattr(self, "_uid", 0) + 1
        return es.enter_context(self.nc.sbuf_tensor(f"sb{self._uid}_{name}", shape, dtype))

    def build(self):
        nc, S = self.nc, self.S
        es = self.es
        self.x = self.sb(es, "x", [128, NT, D], F32)
        self.xT = self.sb(es, "xT", [128, DC, SEQ], BF16)
        self.psA = es.enter_context(nc.psum_tensor("psA", [128, 8, 512], F32))
        self.psB = self.psA[:].bitcast(BF16)
        self.ident_f = self.sb(es, "ident_f", [128, 128], F32)
        self.ident_b = self.sb(es, "ident_b", [128, 128], BF16)
        self.tri_b = self.sb(es, "tri_b", [128, 128], BF16)
        self.ustr_b = self.sb(es, "ustr_b", [128, 128], BF16)
        self.ones_b = self.sb(es, "ones_b", [128, 128], BF16)
        self.slotbase = self.sb(es, "slotbase", [128, 64], F32)
        self.eps_t = self.sb(es, "eps_t", [128, 1], F32)
        self.zero_b = self.sb(es, "zero_b", [128, D], BF16)
        self.mhalf = self.sb(es, "mhalf", [128, NT], F32)
        self.lng = self.sb(es, "lng", [128, D], F32)
        self.lnb = self.sb(es, "lnb", [128, D], F32)
        self.lnst = self.sb(es, "lnst", [128, NT, 2, 6], F32)
        self.lnmv = self.sb(es, "lnmv", [128, NT, 4], F32)

        S.dma("sp", lambda: nc.sync.dma_start(out=self.ident_f[:], in_=self.c["ident_f"]), writes=["ident_f"])
        S.dma("pool", lambda: nc.gpsimd.dma_start(out=self.ident_b[:], in_=self.c["ident_f"]), writes=["ident_b"])
        S.dma("pool", lambda: nc.gpsimd.dma_start(out=self.tri_b[:], in_=self.c["tri"]), writes=["tri_b"])
        S.dma("pool", lambda: nc.gpsimd.dma_start(out=self.ustr_b[:], in_=self.c["ustr"]), writes=["ustr_b"])
        S.dma("sp", lambda: nc.sync.dma_start(out=self.slotbase[:], in_=self.c["slotbase"]), writes=["slotbase"])
        S.op("dve", lambda: nc.vector.memset(self.ones_b[:], 1.0), writes=["ones_b"])
        S.op("dve", lambda: nc.vector.memset(self.eps_t[:], EPS), writes=["eps_t"])
        S.op("dve", lambda: nc.vector.memset(self.zero_b[:], 0.0), writes=["zero_b"])
        S.op("dve", lambda: nc.vector.memset(self.mhalf[:], -0.5), writes=["mhalf"])
        for t in range(NT):
            S.dma("sp", lambda t=t: nc.sync.dma_start(out=self.x[:, t, :], in_=self.x_in[t * 128:(t + 1) * 128, :]),
                  writes=[("x", t)])
        self.need_zero_init = any(k == "moe" for k, _ in self.stages)
        if self.need_zero_init and self.stages[0][0] == "moe":
            self.emit_zero_init([])
        first = True
        for st in self.stages:
            kind, i = st
            if kind in ("moba", "gmlp"):
                if first:
                    for t in range(NT):
                        self.make_xT(t)
                if kind == "moba":
                    self.moba_phase(i)
                else:
                    self.gmlp_phase(i)
            else:
                idx = self.stages.index(st)
                self.moe_phase(i, make_xT_after=(idx + 1 < len(self.stages)))
            first = False
        for t in range(NT):
            S.dma("sp", lambda t=t: nc.sync.dma_start(out=self.out[t * 128:(t + 1) * 128, :], in_=self.x[:, t, :]),
                  reads=[("x", t)])
        S.finish("sp")
        return nc

    def emit_zero_init(self, after_keys):
        nc, S = self.nc, self.S
        if not self.need_zero_init:
            return
        self.need_zero_init = False
        zt = self.zero_b
        for q in range(4):
            S.dma("sp", lambda q=q: nc.sync.dma_start(
                out=self.xg_d[q * 2048:(q + 1) * 2048, :].rearrange("(e p) d -> p e d", p=128),
                in_=zt[:, :].unsqueeze(1).to_broadcast([128, 16, D])),
                reads=["zero_b"] + list(after_keys), writes=[("xgd_init", q)])

    def bank(self, b):
        return self.psA[:, b, :]

    def make_xT(self, t, banks=(6, 7)):
        nc, S = self.nc, self.S
        for hb in range(2):
            b = banks[hb]
            S.op("pe", [lambda c=c, b=b: nc.tensor.transpose(out=self.psA[:, b, (c % 4) * 128:(c % 4 + 1) * 128],
                                                              in_=self.x[:, t, c * 128:(c + 1) * 128],
                                                              identity=self.ident_f[:])
                        for c in range(hb * 4, hb * 4 + 4)],
                 reads=[("x", t), "ident_f"], writes=[("ps", b)])
            S.op("act", lambda b=b, hb=hb: nc.scalar.copy(out=self.xT[:, hb * 4:hb * 4 + 4, t * 128:(t + 1) * 128],
                                                           in_=self.psA[:, b, :].rearrange("p (c n) -> p c n", c=4)),
                 reads=[("ps", b)], writes=[("xT", t)])

    def load_ln(self, gname, bname, i):
        nc, S = self.nc, self.S
        S.dma("sp", lambda: nc.sync.dma_start(out=self.lng[:], in_=self.w[gname][i:i + 1, :].partition_broadcast(128)),
              writes=["lng"])
        S.dma("sp", lambda: nc.sync.dma_start(out=self.lnb[:], in_=self.w[bname][i:i + 1, :].partition_broadcast(128)),
              writes=["lnb"])

    def layer_norm_tiles(self, tiles):
        nc, S = self.nc, self.S
        st, mv = self.lnst, self.lnmv
        t0, n = tiles[0], len(tiles)
        assert tiles == list(range(t0, t0 + n))
        for t in tiles:
            S.op("dve", [lambda h=h, t=t: nc.vector.bn_stats(out=st[:, t, h, :], in_=self.x[:, t, h * 512:(h + 1) * 512])
                         for h in range(2)], reads=[("x", t)], writes=[("lnst", t)])
        for t in tiles:
            S.op("dve", lambda t=t: nc.vector.bn_aggr(out=mv[:, t, 0:2], in_=st[:, t, :, :].rearrange("p a b -> p (a b)")),
                 reads=[("lnst", t)], writes=[("lnmv01", t)])
        k01 = [("lnmv01", t) for t in tiles]
        k2 = [("lnmv2", t) for t in tiles]
        k3 = [("lnmv3", t) for t in tiles]
        S.op("pool", lambda: nc.gpsimd.tensor_scalar(out=mv[:, t0:t0 + n, 2], in0=mv[:, t0:t0 + n, 1], scalar1=EPS, scalar2=None,
                                                     op0=ALU.add), reads=k01, writes=k2)
        S.op("pool", lambda: nc.gpsimd.tensor_tensor(out=mv[:, t0:t0 + n, 2], in0=mv[:, t0:t0 + n, 2], in1=self.mhalf[:, 0:n],
                                                     op=ALU.pow), reads=k2 + ["mhalf"], writes=k2)
        S.op("dve", lambda: nc.vector.scalar_tensor_tensor(out=mv[:, t0:t0 + n, 3], in0=mv[:, t0:t0 + n, 0], scalar=-1.0,
                                                           in1=mv[:, t0:t0 + n, 2], op0=ALU.mult, op1=ALU.mult),
             reads=k01 + k2, writes=k3)
        for t in tiles:
            S.op("act", lambda t=t: nc.scalar.activation(out=self.x[:, t, :], in_=self.x[:, t, :], func=AF.Identity,
                                                         bias=mv[:, t, 3:4], scale=mv[:, t, 2:3]),
                 reads=[("x", t), ("lnmv2", t), ("lnmv3", t)], writes=[("x", t)])
        for t in tiles:
            S.op("dve", lambda t=t: nc.vector.tensor_tensor(out=self.x[:, t, :], in0=self.x[:, t, :], in1=self.lng[:], op=ALU.mult),
                 reads=[("x", t), "lng"], writes=[("x", t)])
        for t in tiles:
            S.op("dve", lambda t=t: nc.vector.tensor_tensor(out=self.x[:, t, :], in0=self.x[:, t, :], in1=self.lnb[:], op=ALU.add),
                 reads=[("x", t), "lnb"], writes=[("x", t)])

    def layer_norm_tile(self, t):
        self.layer_norm_tiles([t])

    def scale_x(self, t):
        nc, S = self.nc, self.S
        S.op("act", lambda: nc.scalar.mul(out=self.x[:, t, :], in_=self.x[:, t, :], mul=ALPHA),
             reads=[("x", t)], writes=[("x", t)])

    def gmlp_phase(self, i):
        nc, S = self.nc, self.S
        j = i // 2
        W = self.w
        TS = 8
        S.barrier()
        with ExitStack() as es:
            v = self.sb(es, "g_v", [128, TS, DV], BF16)
            wv = [self.sb(es, f"g_wv{k}", [128, 8, 384], BF16) for k in range(2)]
            bv = [self.sb(es, f"g_bv{k}", [128, 384], F32) for k in range(2)]
            wo = [self.sb(es, f"g_wo{k}", [128, 3, D], BF16) for k in range(2)]
            tmp = [self.sb(es, f"g_tmp{k}", [128, 384], F32) for k in range(2)]
            stats = self.sb(es, "g_stats", [128, TS, 8, 6], F32)
            mv = self.sb(es, "g_mv", [128, TS, 4], F32)
            lvg = self.sb(es, "g_lvg", [128, DV], BF16)
            lvb = self.sb(es, "g_lvb", [128, DV], BF16)
            wsT = self.sb(es, "g_wsT", [128, 8, 128], BF16)
            bsT = self.sb(es, "g_bsT", [128, 8], F32)
            u = [self.sb(es, f"g_u{k}", [128, 384], BF16) for k in range(2)]
            gate = [self.sb(es, f"g_gate{k}", [128, 384], BF16) for k in range(2)]
            gateT = [self.sb(es, f"g_gateT{k}", [128, 3, 128], BF16) for k in range(2)]

            self.load_ln("ln1_g", "ln1_b", i)
            S.dma("pool", lambda: nc.gpsimd.dma_start(out=lvg[:], in_=W["gmlp_ln_g"][j:j + 1, :].partition_broadcast(128)),
                  writes=["lvg"])
            S.dma("pool", lambda: nc.gpsimd.dma_start(out=lvb[:], in_=W["gmlp_ln_b"][j:j + 1, :].partition_broadcast(128)),
                  writes=["lvb"])
            S.dma("pool", lambda: nc.gpsimd.dma_start(out=wsT[:], in_=W["gmlp_w_sT"][j]), writes=["wsT"])
            S.dma("sp", lambda: nc.sync.dma_start(out=bsT[:], in_=W["gmlp_b_sT"][j]), writes=["bsT"])
            for g in range(8):
                S.op("dve", lambda g=g: nc.vector.tensor_tensor(out=wsT[:, g, :], in0=wsT[:, g, :], in1=self.tri_b[:],
                                                                op=ALU.mult),
                     reads=["wsT", "tri_b"], writes=["wsT"])
            for t in range(NT):
                self.scale_x(t)

            cnt = 0
            for st in range(NT // TS):
                for k in range(8):
                    wb = k % 2
                    c0 = DV + k * 384
                    S.dma("pool", lambda wb=wb, c0=c0: nc.gpsimd.dma_start(
                        out=wv[wb][:], in_=W["gmlp_w_in"][j, :, c0:c0 + 384].rearrange("(c p) f -> p c f", p=128)),
                        writes=[("wv", wb)])
                    S.dma("sp", lambda wb=wb, c0=c0: nc.sync.dma_start(
                        out=bv[wb][:], in_=W["gmlp_b_in"][j:j + 1, c0:c0 + 384].partition_broadcast(128)),
                        writes=[("bv", wb)])
                    for tt in range(TS):
                        t = st * TS + tt
                        b = cnt % 4
                        tb = cnt % 2
                        cnt += 1
                        S.op("pe", [lambda c=c, b=b, t=t, wb=wb: nc.tensor.matmul(
                            self.psA[:, b, 0:384], lhsT=self.xT[:, c, t * 128:(t + 1) * 128], rhs=wv[wb][:, c, :],
                            start=(c == 0), stop=(c == 7)) for c in range(8)],
                            reads=[("xT", t), ("wv", wb)], writes=[("ps", b)])
                        S.op("dve", lambda b=b, tb=tb, wb=wb: nc.vector.tensor_tensor(
                            out=tmp[tb][:], in0=self.psA[:, b, 0:384], in1=bv[wb][:], op=ALU.add),
                            reads=[("ps", b), ("bv", wb)], writes=[("tmp", tb)])
                        S.op("act", lambda tb=tb, tt=tt, k=k: nc.scalar.activation(
                            out=v[:, tt, k * 384:(k + 1) * 384], in_=tmp[tb][:], func=AF.Gelu),
                            reads=[("tmp", tb)], writes=[("v", tt, k)])
                        S.op("dve", lambda tt=tt, k=k: nc.vector.bn_stats(
                            out=stats[:, tt, k, :], in_=v[:, tt, k * 384:(k + 1) * 384]),
                            reads=[("v", tt, k)], writes=[("stats", tt, k)])
                if st == 0:
                    self.emit_zero_init([("v", 0, 0)])
                for tt in range(TS):
                    S.op("dve", lambda tt=tt: nc.vector.bn_aggr(out=mv[:, tt, 0:2],
                                                                in_=stats[:, tt, :, :].rearrange("p a b -> p (a b)")),
                         reads=[("stats", tt, k) for k in range(8)], writes=[("mv", tt)])
                kmv = [("mv", tt) for tt in range(TS)]
                S.op("pool", lambda: nc.gpsimd.tensor_scalar(out=mv[:, :, 2], in0=mv[:, :, 1], scalar1=EPS, scalar2=None, op0=ALU.add),
                     reads=kmv, writes=["mv2"])
                S.op("pool", lambda: nc.gpsimd.tensor_tensor(out=mv[:, :, 2], in0=mv[:, :, 2], in1=self.mhalf[:, 0:TS], op=ALU.pow),
                     reads=["mv2", "mhalf"], writes=["mv2"])
                S.op("dve", lambda: nc.vector.scalar_tensor_tensor(out=mv[:, :, 3], in0=mv[:, :, 0], scalar=-1.0, in1=mv[:, :, 2],
                                                                   op0=ALU.mult, op1=ALU.mult), reads=kmv + ["mv2"], writes=["mv3"])
                for tt in range(TS):
                    vk = [("v", tt, k) for k in range(8)]
                    S.op("act", lambda tt=tt: nc.scalar.activation(out=v[:, tt, :], in_=v[:, tt, :], func=AF.Identity,
                                                                   bias=mv[:, tt, 3:4], scale=mv[:, tt, 2:3]),
                         reads=vk + ["mv2", "mv3"], writes=vk)
                for tt in range(TS):
                    vk = [("v", tt, k) for k in range(8)]
                    S.op("dve", lambda tt=tt: nc.vector.tensor_tensor(out=v[:, tt, :], in0=v[:, tt, :], in1=lvg[:],
                                                                      op=ALU.mult), reads=vk + ["lvg"], writes=vk)
                    S.op("dve", lambda tt=tt: nc.vector.tensor_tensor(out=v[:, tt, :], in0=v[:, tt, :], in1=lvb[:],
                                                                      op=ALU.add), reads=vk + ["lvb"], writes=vk)
                def load_wv(g):
                    wb = g % 2
                    c0 = g * 384
                    S.dma("pool", lambda: nc.gpsimd.dma_start(
                        out=wv[wb][:], in_=W["gmlp_w_in"][j, :, c0:c0 + 384].rearrange("(c p) f -> p c f", p=128)),
                        writes=[("wv", wb)])
                    S.dma("sp", lambda: nc.sync.dma_start(
                        out=bv[wb][:], in_=W["gmlp_b_in"][j:j + 1, c0:c0 + 384].partition_broadcast(128)),
                        writes=[("bv", wb)])

                def load_wo(g):
                    wb = g % 2
                    c0 = g * 384
                    S.dma("pool", lambda: nc.gpsimd.dma_start(
                        out=wo[wb][:], in_=W["gmlp_w_out"][j, c0:c0 + 384, :].rearrange("(c p) f -> p c f", p=128)),
                        writes=[("wo", wb)])

                def p2_A(n):
                    g, tt = n // TS, n % TS
                    t = st * TS + tt
                    wb = g % 2
                    ub = n % 2
                    bu = ub
                    bm = 2 + ub
                    c0 = g * 384
                    S.op("pe", [lambda c=c: nc.tensor.matmul(
                        self.psA[:, bu, 0:384], lhsT=self.xT[:, c, t * 128:(t + 1) * 128], rhs=wv[wb][:, c, :],
                        start=(c == 0), stop=(c == 7)) for c in range(8)],
                        reads=[("xT", t), ("wv", wb)], writes=[("ps", bu)])
                    S.op("dve", lambda: nc.vector.tensor_tensor(
                        out=tmp[ub][:], in0=self.psA[:, bu, 0:384], in1=bv[wb][:], op=ALU.add),
                        reads=[("ps", bu), ("bv", wb)], writes=[("tmp", ub)])
                    S.op("act", lambda: nc.scalar.activation(out=u[ub][:], in_=tmp[ub][:], func=AF.Gelu),
                         reads=[("tmp", ub)], writes=[("u", ub)])
                    S.op("pe", lambda: nc.tensor.matmul(
                        self.psA[:, bm, 0:384], lhsT=wsT[:, g, :], rhs=v[:, tt, c0:c0 + 384], start=True, stop=True),
                        reads=["wsT", ("v", tt, g)], writes=[("ps", bm)])
                    if tt == TS - 1 and g + 2 < 8:
                        load_wv(g + 2)

                def p2_B(n):
                    g, tt = n // TS, n % TS
                    ub = n % 2
                    bm = 2 + ub
                    bt = 4
                    S.op("dve", lambda: nc.vector.scalar_tensor_tensor(
                        out=gate[ub][:], in0=self.psA[:, bm, 0:384], scalar=bsT[:, g:g + 1], in1=u[ub][:],
                        op0=ALU.add, op1=ALU.mult),
                        reads=[("ps", bm), "bsT", ("u", ub)], writes=[("gate", ub)])

                def p2_Bt(n):
                    ub = n % 2
                    bt = 4
                    S.op("pe", [lambda q=q: nc.tensor.transpose(
                        out=self.psB[:, bt, q * 128:(q + 1) * 128], in_=gate[ub][:, q * 128:(q + 1) * 128],
                        identity=self.ident_b[:]) for q in range(3)],
                        reads=[("gate", ub), "ident_b"], writes=[("ps", bt)])
                    S.op("act", lambda: nc.scalar.copy(
                        out=gateT[ub][:], in_=self.psB[:, bt, 0:384].rearrange("p (c n) -> p c n", c=3)),
                        reads=[("ps", bt)], writes=[("gateT", ub)])

                def p2_C(n):
                    g, tt = n // TS, n % TS
                    t = st * TS + tt
                    wb = g % 2
                    ub = n % 2
                    bh = 5
                    S.op("pe", [lambda q=q, hf=hf: nc.tensor.matmul(
                        self.psA[:, bh + hf, :], lhsT=gateT[ub][:, q, :], rhs=wo[wb][:, q, hf * 512:(hf + 1) * 512],
                        start=(q == 0), stop=(q == 2)) for hf in range(2) for q in range(3)],
                        reads=[("gateT", ub), ("wo", wb)], writes=[("ps", bh), ("ps", bh + 1)])
                    S.op("dve", lambda: nc.vector.tensor_tensor(
                        out=self.x[:, t, :], in0=self.psA[:, bh:bh + 2, :].rearrange("p a n -> p (a n)"),
                        in1=self.x[:, t, :], op=ALU.add),
                        reads=[("ps", bh), ("ps", bh + 1), ("x", t)], writes=[("x", t)])
                    if tt == TS - 1 and g + 2 < 8:
                        load_wo(g + 2)

                for g in range(2):
                    load_wv(g)
                    load_wo(g)
                NST = 8 * TS
                for n in range(NST + 2):
                    if 0 <= n - 1 < NST:
                        p2_B(n - 1)
                    if n < NST:
                        p2_A(n)
                    if 0 <= n - 2 < NST:
                        p2_C(n - 2)
                    if 0 <= n - 1 < NST:
                        p2_Bt(n - 1)
            for t0 in range(0, NT, 4):
                self.layer_norm_tiles(list(range(t0, t0 + 4)))
            S.barrier()

    def moba_phase(self, i):
        nc, S = self.nc, self.S
        W = self.w
        j = i // 2
        S.barrier()
        with ExitStack() as es:
            OT = self.sb(es, "a_OT", [128, 8, SEQ], BF16)
            QA = [self.sb(es, f"a_QA{h}", [128, SEQ], BF16) for h in range(2)]
            KA = [self.sb(es, f"a_KA{h}", [128, SEQ], BF16) for h in range(2)]
            VA = [self.sb(es, f"a_VA{h}", [128, NT, 128], BF16) for h in range(2)]
            Wq = [self.sb(es, f"a_Wq{k}", [128, 8, 128], BF16) for k in range(2)]
            Wk = [self.sb(es, f"a_Wk{k}", [128, 8, 128], BF16) for k in range(2)]
            Wv = [self.sb(es, f"a_Wv{k}", [128, 8, 128], BF16) for k in range(2)]
            Wo = self.sb(es, "a_Wo", [128, 8, D], BF16)
            NPT = 4
            PT = [self.sb(es, f"a_PT{k}", [128, 512], BF16) for k in range(NPT)]
            rden = [self.sb(es, f"a_rden{k}", [128, 512], F32) for k in range(2)]
            kms = [self.sb(es, f"a_kms{h}", [64, 8], F32) for h in range(2)]
            kmb = [self.sb(es, f"a_kmb{h}", [128, 8], BF16) for h in range(2)]
            bsw = self.sb(es, "a_bsw", [128, 16, 8], F32)
            bsw2 = self.sb(es, "a_bsw2", [128, 16, 8], F32)
            beq = self.sb(es, "a_beq", [128, 16, 8], F32)
            bmx = self.sb(es, "a_bmx", [128, 16], F32)
            bpw = self.sb(es, "a_bpw", [128, 16, 72], BF16)

            for h in range(2):
                S.op("dve", lambda h=h: nc.vector.memset(QA[h][64:128, :], 0.0), writes=[("QApad", h)])
                S.op("dve", lambda h=h: nc.vector.memset(KA[h][64:128, :], 0.0), writes=[("KApad", h)])
                S.op("dve", lambda h=h: nc.vector.memset(kmb[h][:], 0.0), writes=[("kmb", h)])
            self.load_ln("ln1_g", "ln1_b", i)
            S.dma("pool", lambda: nc.gpsimd.dma_start(out=Wo[:], in_=W["moba_w_o"][j].rearrange("(c p) f -> p c f", p=128)),
                  writes=["Wo"])
            for h in range(2):
                if "noblk" in DBG:
                    continue
                for hh in range(2):
                    S.dma("pool", lambda h=h, hh=hh: nc.gpsimd.dma_start(out=KA[h][64:72, hh * 1024:(hh + 1) * 1024],
                                                                        in_=self.c["blkind"][:, hh * 1024:(hh + 1) * 1024]),
                          reads=[("KApad", h)], writes=[("KAb", h, hh)])

            S.op("dve", lambda: nc.vector.memset(bpw[:], 0.0), writes=["bpw"])
            S.op("dve", lambda: nc.vector.memset(VA[0][:, :, 64:128], 1.0), writes=[("VAones", 0)])
            S.op("dve", lambda: nc.vector.memset(VA[1][:, :, 0:64], 1.0), writes=[("VAones", 1)])
            for t in range(NT):
                self.scale_x(t)

            def load_pair_w(p):
                b = p % 2
                for nm, dst, off in (("Wq", Wq, 0), ("Wk", Wk, D), ("Wv", Wv, 2 * D)):
                    S.dma("pool", lambda dst=dst, off=off: nc.gpsimd.dma_start(
                        out=dst[b][:], in_=W["moba_w_qkv"][j, :, off + p * 128: off + (p + 1) * 128].rearrange("(c p) f -> p c f", p=128)),
                        writes=[(nm, b)])

            if STOP == 1:
                return
            load_pair_w(0)
            bpc = 0
            for p in range(8):
                wb = p % 2
                if p + 1 < 8:
                    load_pair_w(p + 1)
                if STOP == 5:
                    return
                for tq in range(4):
                    cols = slice(tq * 512, (tq + 1) * 512)
                    xk = [("xT", t) for t in range(tq * 4, tq * 4 + 4)]
                    bq = 5
                    S.op("pe", [lambda c=c: nc.tensor.matmul(self.psA[:, bq, :], lhsT=Wq[wb][:, c, :], rhs=self.xT[:, c, cols],
                                                             start=(c == 0), stop=(c == 7)) for c in range(8)],
                         reads=xk + [("Wq", wb)], writes=[("ps", bq)])
                    for h in range(2):
                        S.op("act", lambda h=h: nc.scalar.mul(out=QA[h][0:64, cols], in_=self.psA[h * 64:(h + 1) * 64, bq, :], mul=0.125),
                             reads=[("ps", bq)], writes=[("QA", h, t) for t in range(tq * 4, tq * 4 + 4)])
                    if STOP == 6:
                        return
                    bk = 6
                    S.op("pe", [lambda c=c: nc.tensor.matmul(self.psA[:, bk, :], lhsT=Wk[wb][:, c, :], rhs=self.xT[:, c, cols],
                                                             start=(c == 0), stop=(c == 7)) for c in range(8)],
                         reads=xk + [("Wk", wb)], writes=[("ps", bk)])
                    for h in range(2):
                        S.op("act", lambda h=h: nc.scalar.copy(out=KA[h][0:64, cols], in_=self.psA[h * 64:(h + 1) * 64, bk, :]),
                             reads=[("ps", bk)], writes=[("KA", h, t) for t in range(tq * 4, tq * 4 + 4)])
                    for h in range(2):
                        S.op("dve", lambda h=h: nc.vector.tensor_reduce(
                            out=kms[h][:, 2 * tq:2 * tq + 2],
                            in_=self.psA[h * 64:(h + 1) * 64, bk, :].rearrange("p (a n) -> p a n", a=2), axis=AX.X, op=ALU.add),
                            reads=[("ps", bk), ("KA", 0, tq * 4), ("KA", 1, tq * 4)], writes=[("kms", h)])
                if STOP == 2:
                    return
                for h in range(2):
                    S.op("dve", lambda h=h: nc.vector.tensor_copy(out=kmb[h][0:64, :], in_=kms[h][:]),
                         reads=[("kms", h)], writes=[("kmb", h)])
                for g4 in range(4):
                    bv_ = 7
                    S.op("pe", [lambda c=c, tl=tl: nc.tensor.matmul(
                        self.psA[:, bv_, tl * 128:(tl + 1) * 128], lhsT=self.xT[:, c, (g4 * 4 + tl) * 128:(g4 * 4 + tl + 1) * 128],
                        rhs=Wv[wb][:, c, :], start=(c == 0), stop=(c == 7)) for tl in range(4) for c in range(8)],
                        reads=[("xT", t) for t in range(g4 * 4, g4 * 4 + 4)] + [("Wv", wb)], writes=[("ps", bv_)])
                    pv = self.psA[:, bv_, :].rearrange("p (a n) -> p a n", a=4)
                    S.op("act", lambda: nc.scalar.copy(out=VA[0][:, g4 * 4:g4 * 4 + 4, 0:64], in_=pv[:, :, 0:64]),
                         reads=[("ps", bv_)], writes=[("VA", 0, t) for t in range(g4 * 4, g4 * 4 + 4)])
                    S.op("act", lambda: nc.scalar.copy(out=VA[1][:, g4 * 4:g4 * 4 + 4, 64:128], in_=pv[:, :, 64:128]),
                         reads=[("ps", bv_)], writes=[("VA", 1, t) for t in range(g4 * 4, g4 * 4 + 4)])
                if STOP == 3:
                    return
                if "nobias" not in DBG:
                    S.op("pe", [lambda h=h, qt=qt: nc.tensor.matmul(
                        self.psA[:, 7, (qt - 8) * 16 + h * 8:(qt - 8) * 16 + (h + 1) * 8], lhsT=QA[h][:, qt * 128:(qt + 1) * 128],
                        rhs=kmb[h][:, :], start=True, stop=True) for qt in range(8, 16) for h in range(2)],
                        reads=[("QA", h, qt) for h in range(2) for qt in range(8, 16)] +
                              [("kmb", 0), ("kmb", 1), ("QApad", 0), ("QApad", 1)] +
                              [("QAb", h, qt) for h in range(2) for qt in range(8, 16)], writes=[("ps", 7)])
                    S.op("dve", lambda: nc.vector.memset(bsw[:], -1.0e30), writes=["bsw"])
                    psv = self.psA[:, 7, 0:128].rearrange("p (q h e) -> p q h e", q=8, h=2)
                    for cur in range(4, 8):
                        q0 = 2 * (cur - 4)
                        S.op("dve", lambda cur=cur, q0=q0: nc.vector.tensor_copy(
                            out=bsw[:].rearrange("p (q h) e -> p q h e", h=2)[:, q0:q0 + 2, :, 0:cur], in_=psv[:, q0:q0 + 2, :, 0:cur]),
                            reads=[("ps", 7)], writes=["bsw"])
                    S.op("dve", lambda: nc.vector.tensor_copy(out=bsw2[:], in_=bsw[:]), reads=["bsw"], writes=["bsw2"])
                    for rnd in range(2):
                        S.op("dve", lambda: nc.vector.tensor_reduce(out=bmx[:], in_=bsw2[:], axis=AX.X, op=ALU.max),
                             reads=["bsw2"], writes=["bmx"])
                        S.op("dve", lambda: nc.vector.tensor_tensor(out=beq[:], in0=bsw2[:], in1=bmx[:].unsqueeze(2).to_broadcast([128, 16, 8]),
                                                                    op=ALU.is_equal), reads=["bsw2", "bmx"], writes=["beq"])
                        S.op("dve", lambda: nc.vector.scalar_tensor_tensor(
                            out=bsw2[:].rearrange("p g e -> p (g e)"), in0=beq[:].rearrange("p g e -> p (g e)"), scalar=-3.0e30,
                            in1=bsw2[:].rearrange("p g e -> p (g e)"), op0=ALU.mult, op1=ALU.add),
                            reads=["beq", "bsw2"], writes=["bsw2"])
                    S.op("dve", lambda: nc.vector.tensor_reduce(out=bmx[:], in_=bsw2[:], axis=AX.X, op=ALU.max),
                         reads=["bsw2"], writes=["bmx"])
                    S.op("dve", lambda: nc.vector.tensor_tensor(out=beq[:], in0=bsw[:], in1=bmx[:].unsqueeze(2).to_broadcast([128, 16, 8]),
                                                                op=ALU.is_lt), reads=["bsw", "bmx"], writes=["beq"])
                    S.op("dve", lambda: nc.vector.tensor_scalar(out=bpw[:, :, 64:72], in0=beq[:], scalar1=NEG, scalar2=None, op0=ALU.mult),
                         reads=["beq"], writes=["bpw"])
                    for cur in range(4, 8):
                        g0 = 4 * (cur - 4)
                        S.op("dve", lambda cur=cur, g0=g0: nc.vector.memset(bpw[:, g0:g0 + 4, 64 + cur:65 + cur], 0.0),
                             reads=["bpw"], writes=["bpw"])
                    bbanks = {0: (5, 6), 1: (7, 0)}
                    for h in range(2):
                        for half in range(2):
                            bb = bbanks[h][half]
                            S.op("pe", [lambda h=h, qq=qq, bb=bb, half=half: nc.tensor.matmul(
                                self.psA[0:72, bb, qq * 128:(qq + 1) * 128], lhsT=bpw[:, (half * 4 + qq) * 2 + h, 0:72],
                                rhs=self.ident_b[:], start=True, stop=True) for qq in range(4)],
                                reads=["bpw", "ident_b"], writes=[("ps", bb)])
                            S.op("act", lambda h=h, bb=bb, half=half: nc.scalar.copy(
                                out=QA[h][64:72, 1024 + half * 512:1024 + (half + 1) * 512], in_=self.psA[64:72, bb, :]),
                                reads=[("ps", bb)], writes=[("QAb", h, qt) for qt in range(8 + half * 4, 12 + half * 4)])
                if STOP == 4:
                    return
                items = []
                for h in range(2):
                    for Qc in range(4):
                        nkt = 4 * Qc + 4
                        for kt in range(nkt):
                            items.append((h, Qc, kt, nkt))
                state = {"n": 0}

                def emit_S(n):
                    h, Qc, kt, nkt = items[n]
                    ii = kt - 4 * Qc
                    c0 = max(0, ii) * 128
                    sbk = n % 3
                    pb = n % NPT
                    Kd = 128
                    qt0 = 4 * Qc + c0 // 128
                    rd = [("KA", h, kt), ("QApad", h), ("KApad", h), ("KAb", h, 0), ("KAb", h, 1)]
                    rd += [("QA", h, t) for t in range(qt0, 4 * Qc + 4)]
                    if Qc >= 2:
                        rd += [("QAb", h, t) for t in range(qt0, 4 * Qc + 4)]
                    S.op("pe", lambda: nc.tensor.matmul(self.psA[:, sbk, c0:512], lhsT=KA[h][0:Kd, kt * 128:(kt + 1) * 128],
                                                        rhs=QA[h][0:Kd, Qc * 512 + c0:(Qc + 1) * 512], start=True, stop=True),
                         reads=rd, writes=[("ps", sbk)])
                    S.op("act", lambda: nc.scalar.activation(out=PT[pb][:, c0:512], in_=self.psA[:, sbk, c0:512], func=AF.Exp),
                         reads=[("ps", sbk)], writes=[("PT", pb)])
                    if ii >= 0:
                        S.op("pool", lambda: nc.gpsimd.tensor_tensor(out=PT[pb][:, c0:c0 + 128], in0=PT[pb][:, c0:c0 + 128],
                                                                      in1=self.tri_b[:], op=ALU.mult),
                             reads=[("PT", pb), "tri_b"], writes=[("PT", pb)])

                def emit_PV(n):
                    h, Qc, kt, nkt = items[n]
                    ii = kt - 4 * Qc
                    c0 = max(0, ii) * 128
                    pb = n % NPT
                    ob = 3 + ((h * 4 + Qc) % 3)
                    S.op("pe", lambda: nc.tensor.matmul(self.psA[:, ob, c0:512], lhsT=VA[h][:, kt, :], rhs=PT[pb][:, c0:512],
                                                        start=(kt == 0), stop=(kt == nkt - 1)),
                         reads=[("PT", pb), ("VA", h, kt), ("VAones", h)], writes=[("ps", ob)])
                    if kt == nkt - 1:
                        r = (h * 4 + Qc) % 2
                        osl = slice(h * 64, (h + 1) * 64)
                        dsl = slice((1 - h) * 64, (2 - h) * 64)
                        S.op("dve", lambda: nc.vector.reciprocal(out=rden[r][osl, :], in_=self.psA[dsl, ob, :]),
                             reads=[("ps", ob)], writes=[("rden", r)])
                        S.op("dve", lambda: nc.vector.tensor_tensor(out=OT[osl, p, Qc * 512:(Qc + 1) * 512], in0=self.psA[osl, ob, :],
                                                                    in1=rden[r][osl, :], op=ALU.mult),
                             reads=[("ps", ob), ("rden", r)], writes=[("OT", p, h, Qc)])

                LA = 2
                if "noattn" in DBG:
                    items = []
                for n in range(len(items) + LA):
                    if n < len(items):
                        emit_S(n)
                    if n >= LA:
                        emit_PV(n - LA)
                if p == 0:
                    self.emit_zero_init([("OT", 0, 0, 0)])
            otk = [("OT", p, h, Qc) for p in range(8) for h in range(2) for Qc in range(4)]
            for t in range(NT):
                by = 5 + 0
                for hf in range(2):
                    S.op("pe", [lambda p=p, hf=hf: nc.tensor.matmul(self.psA[:, 5 + hf, :], lhsT=OT[:, p, t * 128:(t + 1) * 128],
                                                                    rhs=Wo[:, p, hf * 512:(hf + 1) * 512],
                                                                    start=(p == 0), stop=(p == 7)) for p in range(8)],
                         reads=[("OT", p, h, t // 4) for p in range(8) for h in range(2)] + ["Wo"], writes=[("ps", 5 + hf)])
                S.op("dve", lambda: nc.vector.tensor_tensor(out=self.x[:, t, :], in0=self.psA[:, 5:7, :].rearrange("p a n -> p (a n)"),
                                                            in1=self.x[:, t, :], op=ALU.add),
                     reads=[("ps", 5), ("ps", 6), ("x", t)], writes=[("x", t)])
                if t % 4 == 3:
                    self.layer_norm_tiles(list(range(t - 3, t + 1)))
            S.barrier()

    def moe_phase(self, i, make_xT_after=True):
        nc, S = self.nc, self.S
        W = self.w
        S.barrier()
        with ExitStack() as es:
            wr = self.sb(es, "m_wr", [128, 8, 72], F32)
            br = self.sb(es, "m_br", [128, 72], F32)
            slotf = self.sb(es, "m_slotf", [128, NT, 2], F32)
            sloti = self.sb(es, "m_sloti", [128, NT, 2], I32)
            gates = self.sb(es, "m_gates", [128, NT, 2], F32)
            NB = 4
            NBX = 4
            W13 = [self.sb(es, f"m_w13_{k}", [128, 8, 512], BF16) for k in range(NB)]
            W2 = [self.sb(es, f"m_w2_{k}", [128, 2, D], BF16) for k in range(NB)]

            self.load_ln("ln2_g", "ln2_b", i)
            S.dma("sp", lambda: nc.sync.dma_start(out=wr[:], in_=W["moe_w_r"][i].rearrange("(c p) f -> p c f", p=128)),
                  writes=["wr"])
            S.dma("sp", lambda: nc.sync.dma_start(out=br[:], in_=W["moe_b_r"][i:i + 1, :].partition_broadcast(128)),
                  writes=["br"])

            def load_w13(e):
                b = e % NB
                S.dma("pool", lambda: nc.gpsimd.dma_start(
                    out=W13[b][:, :, 0:256], in_=W["moe_w1"][i, e].rearrange("(c p) f -> p c f", p=128)),
                    writes=[("W13a", b)])
                S.dma("pool", lambda: nc.gpsimd.dma_start(
                    out=W13[b][:, :, 256:512], in_=W["moe_w3"][i, e].rearrange("(c p) f -> p c f", p=128)),
                    writes=[("W13b", b)])

            def load_w2(e):
                b = e % NB
                S.dma("pool", lambda: nc.gpsimd.dma_start(
                    out=W2[b][:], in_=W["moe_w2"][i, e].rearrange("(c p) f -> p c f", p=128)),
                    writes=[("W2", b)])

            for e in range(NB):
                load_w13(e)
                load_w2(e)

            xgd_keys = []
            with ExitStack() as rs:
                xnT = [self.sb(rs, f"m_xnT{k}", [128, 8, 128], F32) for k in range(2)]
                lg = self.sb(rs, "m_lg", [128, NT, 72], F32)
                dd = self.sb(rs, "m_dd", [128, NT, 8], F32)
                pen = self.sb(rs, "m_pen", [128, NT, 8], F32)
                ml = self.sb(rs, "m_ml", [128, NT, 64], F32)
                oh1 = self.sb(rs, "m_oh1", [128, NT, 64], F32)
                oh2 = self.sb(rs, "m_oh2", [128, NT, 64], F32)
                posb = self.sb(rs, "m_posb", [128, NT, 64], F32)
                A_b = self.sb(rs, "m_Ab", [128, NT, 64], BF16)
                sm = self.sb(rs, "m_sm", [128, 8, NT], F32)
                def rt_T(t):
                    k2 = t % 2
                    for hb_ in range(2):
                        b = hb_ if t % 2 == 0 else 6 + hb_
                        S.op("pe", [lambda c=c, b=b: nc.tensor.transpose(
                            out=self.psA[:, b, (c % 4) * 128:(c % 4 + 1) * 128], in_=self.x[:, t, c * 128:(c + 1) * 128],
                            identity=self.ident_f[:]) for c in range(hb_ * 4, hb_ * 4 + 4)],
                            reads=[("x", t), "ident_f"], writes=[("ps", b)])
                        S.op("act", lambda b=b, hb_=hb_: nc.scalar.copy(
                            out=xnT[k2][:, hb_ * 4:hb_ * 4 + 4, :], in_=self.psA[:, b, :].rearrange("p (c n) -> p c n", c=4)),
                            reads=[("ps", b)], writes=[("xnT", k2, hb_)])

                def rt_M(t):
                    k2 = t % 2
                    bl = 2 + (t // 4) % 2
                    S.op("pe", [lambda c=c: nc.tensor.matmul(self.psA[:, bl, (t % 4) * 72:(t % 4 + 1) * 72], lhsT=xnT[k2][:, c, :],
                                                             rhs=wr[:, c, :], start=(c == 0), stop=(c == 7)) for c in range(8)],
                         reads=[("xnT", k2, 0), ("xnT", k2, 1), "wr"], writes=[("ps", bl)])
                    if t % 4 == 3:
                        t0 = t - 3
                        S.op("dve", lambda: nc.vector.tensor_tensor(
                            out=lg[:, t0:t0 + 4, :], in0=self.psA[:, bl, 0:288].rearrange("p (a n) -> p a n", a=4),
                            in1=br[:, :].unsqueeze(1).to_broadcast([128, 4, 72]), op=ALU.add),
                            reads=[("ps", bl), "br"], writes=["lg"])

                for t in range(NT + 1):
                    if t < NT:
                        rt_T(t)
                    if t >= 1:
                        rt_M(t - 1)
                gmax, sume, gp, top1, top2, e2 = (sm[:, k, :] for k in range(6))
                lgg = lg[:, :, 0:8]
                S.op("dve", lambda: nc.vector.tensor_reduce(out=gmax, in_=lgg, axis=AX.X, op=ALU.max), reads=["lg"], writes=["gmax"])
                S.op("dve", lambda: nc.vector.tensor_tensor(out=dd[:], in0=lgg, in1=gmax.unsqueeze(2).to_broadcast([128, NT, 8]),
                                                            op=ALU.subtract), reads=["lg", "gmax"], writes=["dd"])
                S.op("dve", lambda: nc.vector.tensor_scalar(out=pen[:], in0=dd[:], scalar1=0.0, scalar2=-1.0e9,
                                                            op0=ALU.is_lt, op1=ALU.mult), reads=["dd"], writes=["pen"])
                S.op("act", lambda: nc.scalar.activation(out=dd[:], in_=dd[:], func=AF.Exp), reads=["dd", "pen"], writes=["dd"])
                S.op("dve", lambda: nc.vector.tensor_reduce(out=sume, in_=dd[:], axis=AX.X, op=ALU.add), reads=["dd"], writes=["sume"])
                S.op("dve", lambda: nc.vector.reciprocal(out=gp, in_=sume), reads=["sume"], writes=["gp"])
                S.op("dve", lambda: nc.vector.tensor_tensor(
                    out=ml[:].rearrange("p t (g e) -> p t g e", g=8), in0=lg[:, :, 8:72].rearrange("p t (g e) -> p t g e", g=8),
                    in1=pen[:].unsqueeze(3).to_broadcast([128, NT, 8, 8]), op=ALU.add), reads=["lg", "pen"], writes=["ml"])
                S.op("dve", lambda: nc.vector.tensor_reduce(out=top1, in_=ml[:], axis=AX.X, op=ALU.max), reads=["ml"], writes=["top1"])
                S.op("dve", lambda: nc.vector.tensor_tensor(out=oh1[:], in0=ml[:], in1=top1.unsqueeze(2).to_broadcast([128, NT, 64]),
                                                            op=ALU.is_equal), reads=["ml", "top1"], writes=["oh1"])
                S.op("dve", lambda: nc.vector.scalar_tensor_tensor(
                    out=ml[:].rearrange("p t e -> p (t e)"), in0=oh1[:].rearrange("p t e -> p (t e)"), scalar=-1.0e9,
                    in1=ml[:].rearrange("p t e -> p (t e)"), op0=ALU.mult, op1=ALU.add), reads=["oh1", "ml"], writes=["ml"])
                S.op("dve", lambda: nc.vector.tensor_reduce(out=top2, in_=ml[:], axis=AX.X, op=ALU.max), reads=["ml"], writes=["top2"])
                S.op("dve", lambda: nc.vector.tensor_tensor(out=oh2[:], in0=ml[:], in1=top2.unsqueeze(2).to_broadcast([128, NT, 64]),
                                                            op=ALU.is_equal), reads=["ml", "top2"], writes=["oh2"])
                S.op("dve", lambda: nc.vector.tensor_tensor(out=e2, in0=top2, in1=top1, op=ALU.subtract),
                     reads=["top1", "top2"], writes=["e2"])
                S.op("act", lambda: nc.scalar.activation(out=e2, in_=e2, func=AF.Exp), reads=["e2"], writes=["e2"])
                S.op("dve", lambda: nc.vector.tensor_scalar(out=e2, in0=e2, scalar1=1.0, scalar2=None, op0=ALU.add),
                     reads=["e2"], writes=["e2"])
                S.op("dve", lambda: nc.vector.reciprocal(out=e2, in_=e2), reads=["e2"], writes=["e2"])
                S.op("dve", lambda: nc.vector.tensor_tensor(out=gates[:, :, 0], in0=e2, in1=gp, op=ALU.mult),
                     reads=["e2", "gp"], writes=["gates0"])
                S.op("dve", lambda: nc.vector.tensor_tensor(out=gates[:, :, 1], in0=gp, in1=gates[:, :, 0], op=ALU.subtract),
                     reads=["gp", "gates0"], writes=["gates1"])
                S.op("dve", lambda: nc.vector.tensor_tensor(out=A_b[:], in0=oh1[:], in1=oh2[:], op=ALU.add),
                     reads=["oh1", "oh2"], writes=["A_b"])
                for t in range(NT):
                    bp = 4 + t // 8
                    cs = slice((t % 8) * 64, (t % 8 + 1) * 64)
                    mm = [lambda: nc.tensor.matmul(self.psA[:, bp, cs], lhsT=self.ustr_b[:], rhs=A_b[:, t, :], start=True, stop=(t == 0))]
                    for jj in range(t):
                        mm.append(lambda jj=jj: nc.tensor.matmul(self.psA[:, bp, cs], lhsT=self.ones_b[:], rhs=A_b[:, jj, :],
                                                                 start=False, stop=(jj == t - 1)))
                    S.op("pe", mm, reads=["A_b", "ustr_b", "ones_b"], writes=[("ps", bp)])
                S.op("dve", lambda: nc.vector.tensor_tensor(
                    out=posb[:], in0=self.psA[:, 4:6, :].rearrange("p a (t e) -> p (a t) e", e=64),
                    in1=self.slotbase[:, :].unsqueeze(1).to_broadcast([128, NT, 64]), op=ALU.add),
                    reads=[("ps", 4), ("ps", 5), "slotbase"], writes=["posb"])
                S.op("dve", lambda: nc.vector.tensor_tensor(out=oh1[:], in0=oh1[:], in1=posb[:], op=ALU.mult),
                     reads=["oh1", "posb"], writes=["oh1"])
                S.op("dve", lambda: nc.vector.tensor_reduce(out=slotf[:, :, 0], in_=oh1[:], axis=AX.X, op=ALU.add),
                     reads=["oh1"], writes=["slotf0"])
                S.op("dve", lambda: nc.vector.tensor_tensor(out=oh2[:], in0=oh2[:], in1=posb[:], op=ALU.mult),
                     reads=["oh2", "posb"], writes=["oh2"])
                S.op("dve", lambda: nc.vector.tensor_reduce(out=slotf[:, :, 1], in_=oh2[:], axis=AX.X, op=ALU.add),
                     reads=["oh2"], writes=["slotf1"])
                S.op("dve", lambda: nc.vector.tensor_copy(out=sloti[:], in_=slotf[:]),
                     reads=["slotf0", "slotf1"], writes=["sloti"])
                for t in range(NT):
                    for k in range(2):
                        key = ("xgd", t, k)
                        xgd_keys.append(key)
                        S.dma("pool", lambda k=k: nc.gpsimd.indirect_dma_start(
                            out=self.xg_d[:, :], out_offset=bass.IndirectOffsetOnAxis(ap=sloti[:, t, k:k + 1], axis=0),
                            in_=self.x[:, t, :], in_offset=None),
                            reads=[("x", t), "sloti"] + [("xgd_init", q) for q in range(4)], writes=[key])
                    self.scale_x(t)
                S.barrier()

            xg = [self.sb(es, f"m_xg{k}", [128, D], BF16) for k in range(NBX)]
            xgT = [self.sb(es, f"m_xgT{k}", [128, 8, 128], BF16) for k in range(2)]
            hs = [self.sb(es, f"m_hs{k}", [128, 256], F32) for k in range(2)]
            hb = [self.sb(es, f"m_hb{k}", [128, 256], BF16) for k in range(2)]
            hT = [self.sb(es, f"m_hT{k}", [128, 2, 128], BF16) for k in range(2)]
            ysb = [self.sb(es, f"m_ysb{k}", [128, D], BF16) for k in range(2)]

            yd_keys = []

            def st_xg(e):
                bx = e % NBX
                S.dma("sp", lambda: nc.sync.dma_start(out=xg[bx][:], in_=self.xg_d[e * CAP:(e + 1) * CAP, :]),
                      reads=xgd_keys + [("xgd_init", q) for q in range(4)], writes=[("xg", bx)])

            def st_A(e):
                bx = e % NBX
                p2 = e % 2
                S.op("pe", [lambda c=c: nc.tensor.transpose(out=self.psB[:, p2, c * 128:(c + 1) * 128],
                                                            in_=xg[bx][:, c * 128:(c + 1) * 128], identity=self.ident_b[:])
                            for c in range(8)], reads=[("xg", bx), "ident_b"], writes=[("ps", p2)])
                S.op("act", lambda: nc.scalar.copy(out=xgT[p2][:], in_=self.psB[:, p2, :].rearrange("p (c n) -> p c n", c=8)),
                     reads=[("ps", p2)], writes=[("xgT", p2)])

            def st_B(e):
                b = e % NB
                p2 = e % 2
                S.op("pe", [lambda c=c: nc.tensor.matmul(self.psA[:, 2 + p2, :], lhsT=xgT[p2][:, c, :], rhs=W13[b][:, c, :],
                                                         start=(c == 0), stop=(c == 7)) for c in range(8)],
                     reads=[("xgT", p2), ("W13a", b), ("W13b", b)], writes=[("ps", 2 + p2)])
                S.op("act", lambda: nc.scalar.activation(out=hs[p2][:], in_=self.psA[:, 2 + p2, 0:256], func=AF.Silu),
                     reads=[("ps", 2 + p2)], writes=[("hs", p2)])
                S.op("dve", lambda: nc.vector.tensor_tensor(out=hb[p2][:], in0=self.psA[:, 2 + p2, 256:512], in1=hs[p2][:],
                                                            op=ALU.mult), reads=[("ps", 2 + p2), ("hs", p2)], writes=[("hb", p2)])
                if e + NB < 64:
                    load_w13(e + NB)

            def st_C(e):
                p2 = e % 2
                S.op("pe", [lambda q=q: nc.tensor.transpose(out=self.psB[:, 4, q * 128:(q + 1) * 128],
                                                            in_=hb[p2][:, q * 128:(q + 1) * 128], identity=self.ident_b[:])
                            for q in range(2)], reads=[("hb", p2), "ident_b"], writes=[("ps", 4)])
                S.op("act", lambda: nc.scalar.copy(out=hT[p2][:], in_=self.psB[:, 4, 0:256].rearrange("p (c n) -> p c n", c=2)),
                     reads=[("ps", 4)], writes=[("hT", p2)])

            def st_D(e):
                b = e % NB
                p2 = e % 2
                by = 5
                S.op("pe", [lambda q=q, hf=hf: nc.tensor.matmul(self.psA[:, by + hf, :], lhsT=hT[p2][:, q, :],
                                                                rhs=W2[b][:, q, hf * 512:(hf + 1) * 512],
                                                                start=(q == 0), stop=(q == 1))
                            for hf in range(2) for q in range(2)],
                     reads=[("hT", p2), ("W2", b)], writes=[("ps", by), ("ps", by + 1)])
                S.op("dve", lambda: nc.vector.tensor_copy(out=ysb[p2][:], in_=self.psA[:, by:by + 2, :].rearrange("p a n -> p (a n)")),
                     reads=[("ps", by), ("ps", by + 1)], writes=[("ysb", p2)])
                key = ("yd", e)
                yd_keys.append(key)
                S.dma("sp", lambda: nc.sync.dma_start(out=self.y_d[e * CAP:(e + 1) * CAP, :], in_=ysb[p2][:]),
                      reads=[("ysb", p2)], writes=[key])
                if e + NB < 64:
                    load_w2(e + NB)

            st_xg(0)
            st_xg(1)
            for s_ in range(64 + 3):
                if s_ + 2 < 64:
                    st_xg(s_ + 2)
                if s_ < 64:
                    st_A(s_)
                if 0 <= s_ - 1 < 64:
                    st_B(s_ - 1)
                if 0 <= s_ - 2 < 64:
                    st_C(s_ - 2)
                if 0 <= s_ - 3 < 64:
                    st_D(s_ - 3)

            NG = 8
            gbuf = [W13[k // 4][:, (k % 4) * 2:(k % 4) * 2 + 2, :].rearrange("p a n -> p (a n)") for k in range(NG)]
            wkeys = [("W13a", 0), ("W13b", 0), ("W13a", 1), ("W13b", 1)]

            def gather(t):
                for k in range(2):
                    gi = (2 * t + k) % NG
                    S.dma("pool", lambda k=k, gi=gi: nc.gpsimd.indirect_dma_start(
                        out=gbuf[gi], out_offset=None, in_=self.y_d[:, :],
                        in_offset=bass.IndirectOffsetOnAxis(ap=sloti[:, t, k:k + 1], axis=0)),
                        reads=yd_keys + ["sloti"], writes=[("gb", gi)])

            S.acquire("pool", wkeys)
            for t in range(4):
                gather(t)
            for t0 in range(0, NT, 2):
                for t in (t0, t0 + 1):
                    for k in range(2):
                        gi = (2 * t + k) % NG
                        S.op("dve", lambda t=t, k=k, gi=gi: nc.vector.scalar_tensor_tensor(
                            out=self.x[:, t, :], in0=gbuf[gi], scalar=gates[:, t, k:k + 1], in1=self.x[:, t, :],
                            op0=ALU.mult, op1=ALU.add),
                            reads=[("gb", gi), "gates0", "gates1", ("x", t)], writes=[("x", t)])
                for t in (t0 + 4, t0 + 5):
                    if t < NT:
                        gather(t)
                self.layer_norm_tiles([t0, t0 + 1])
                if make_xT_after:
                    self.make_xT(t0)
                    self.make_xT(t0 + 1, banks=(4, 5))
            S.barrier()


def prep_inputs(inputs):
    shared = {}
    for n in ("moba_w_qkv", "moba_w_o", "gmlp_w_in", "gmlp_b_in", "gmlp_ln_g", "gmlp_ln_b", "gmlp_w_out",
              "ln1_g", "ln1_b", "ln2_g", "ln2_b", "moe_w1", "moe_w3", "moe_w2"):
        shared[n] = np.ascontiguousarray(inputs[n], dtype=np.float32)
    shared["gmlp_w_sT"] = np.ascontiguousarray(np.transpose(inputs["gmlp_w_s"], (0, 3, 1, 2)))
    shared["gmlp_b_sT"] = np.ascontiguousarray(np.transpose(inputs["gmlp_b_s"], (0, 2, 1)))
    shared["moe_w_r"] = np.ascontiguousarray(np.concatenate([inputs["moe_w_grp"], inputs["moe_w_rt"]], axis=2))
    shared["moe_b_r"] = np.ascontiguousarray(np.concatenate([inputs["moe_b_grp"], inputs["moe_b_rt"]], axis=1))
    shared.update(host_consts())
    return shared


FULL_STAGES = [("moba", 0), ("moe", 0), ("gmlp", 1), ("moe", 1), ("moba", 2), ("moe", 2), ("gmlp", 3), ("moe", 3)]


def run(inputs, stages, n_cores=8, trace=False, strict_same=True):
    shared = prep_inputs(inputs)
    kb = K(stages, strict_same=strict_same)
    nc = kb.build()
    x = np.ascontiguousarray(inputs["x"], dtype=np.float32)
    in_maps = []
    for c in range(n_cores):
        m = dict(shared)
        m["x"] = x[c]
        in_maps.append(m)
    res = run_bass_kernel_spmd(nc, in_maps, core_ids=list(range(n_cores)), trace=trace)
    out = np.stack([r["out"] for r in res.results], axis=0)
    return out, res


def kernel(**inputs):
    out, _ = run(inputs, FULL_STAGES, n_cores=8)
    return out.astype(np.float32)
```

```python
import numpy as np
import ml_dtypes
from contextlib import ExitStack
import concourse.bass as bass
import concourse.mybir as mybir
from concourse.bass_utils import run_bass_kernel_spmd

F32 = mybir.dt.float32
BF16 = mybir.dt.bfloat16
I32 = mybir.dt.int32
AF = mybir.ActivationFunctionType
ALU = mybir.AluOpType
AX = mybir.AxisListType

D = 1024
SEQ = 2048
NT = 16
DC = 8
DEPTH = 4
ALPHA = float(8.0 ** 0.25)
EPS = 1e-5
NEG = -30000.0
CAP = 128
NSLOT = 64 * CAP
DV = 3072
import os
DBG = set(os.environ.get('KDBG', '').split(','))
STOP = int(os.environ.get('KSTOP', '0'))


class Sched:
    R = 16

    def __init__(self, nc, strict_same=True):
        self.nc = nc
        self.strict_same = strict_same
        self.eng = {"pe": nc.tensor, "act": nc.scalar, "dve": nc.vector, "pool": nc.gpsimd, "sp": nc.sync}
        self.sem = {}
        self.cnt = {}
        self.seen = {k: {} for k in self.eng}
        self.stack = ExitStack()
        for k in self.eng:
            self.sem[k] = self.stack.enter_context(nc.semaphore("sem_" + k))
            self.cnt[k] = 0
        self.rings = {}
        for k in ("sp", "pool", "act"):
            self.rings[k] = {"sems": [self.stack.enter_context(nc.semaphore(f"ring_{k}_{i}")) for i in range(self.R)],
                             "n": 0}
        self.lastw = {}
        self.readers = {}

    def _wait(self, qn, tok):
        if tok is None:
            return
        kind, src, idx = tok
        if kind == "c":
            if src == qn and (qn == "pe" or not self.strict_same):
                return
            key = ("c", src)
            val = idx
            sem = self.sem[src]
        else:
            ring = self.rings[src]
            slot = idx % self.R
            key = ("d", src, slot)
            val = 16 * (idx // self.R + 1)
            sem = ring["sems"][slot]
        if self.seen[qn].get(key, 0) >= val:
            return
        self.eng[qn].wait_ge(sem, val)
        self.seen[qn][key] = val

    def _deps(self, reads, writes):
        deps = []
        for r in reads:
            t = self.lastw.get(r)
            if t is not None:
                deps.append(t)
        for w in writes:
            t = self.lastw.get(w)
            if t is not None:
                deps.append(t)
            deps.extend(self.readers.get(w, ()))
        return deps

    def _commit(self, tok, reads, writes):
        for r in reads:
            self.readers.setdefault(r, []).append(tok)
        for w in writes:
            self.lastw[w] = tok
            self.readers[w] = []

    def op(self, qn, fns, reads=(), writes=()):
        for t in self._deps(reads, writes):
            self._wait(qn, t)
        if callable(fns):
            fns = [fns]
        inst = None
        for f in fns:
            inst = f()
        inst.then_inc(self.sem[qn], 1)
        self.cnt[qn] += 1
        tok = ("c", qn, self.cnt[qn])
        self._commit(tok, reads, writes)
        return tok

    def dma(self, qn, fn, reads=(), writes=()):
        ring = self.rings[qn]
        i = ring["n"]
        if i >= self.R:
            self._wait(qn, ("d", qn, i - self.R))
        for t in self._deps(reads, writes):
            self._wait(qn, t)
        inst = fn()
        inst.then_inc(ring["sems"][i % self.R], 16)
        ring["n"] += 1
        tok = ("d", qn, i)
        self._commit(tok, reads, writes)
        return tok

    def acquire(self, qn, keys):
        for t in self._deps((), keys):
            self._wait(qn, t)

    def barrier(self):
        toks = []
        for k in self.eng:
            if self.cnt[k] > 0:
                toks.append(("c", k, self.cnt[k]))
        for k, ring in self.rings.items():
            n = ring["n"]
            for i in range(max(0, n - self.R), n):
                toks.append(("d", k, i))
        for qn in self.eng:
            for t in toks:
                if t[0] == "c" and t[1] == qn:
                    continue
                self._wait(qn, t)

    def finish(self, qn="sp"):
        toks = []
        for k, ring in self.rings.items():
            n = ring["n"]
            for i in range(max(0, n - self.R), n):
                toks.append(("d", k, i))
        for k in self.eng:
            if self.cnt[k] > 0 and k != qn:
                toks.append(("c", k, self.cnt[k]))
        for t in toks:
            self._wait(qn, t)


def host_consts():
    c = {}
    c["c_ident_f"] = np.eye(128, dtype=np.float32)
    k = np.arange(128)
    tri = (k[:, None] <= k[None, :]).astype(np.float32)
    ustr = (k[:, None] < k[None, :]).astype(np.float32)
    c["c_tri"] = tri
    c["c_ustr"] = ustr
    blk = (np.arange(SEQ)[None, :] // 256 == np.arange(8)[:, None]).astype(np.float32)
    c["c_blkind"] = blk
    c["c_slotbase"] = np.tile((np.arange(64) * CAP).astype(np.float32)[None, :], (128, 1))
    return c


class K:
    def __init__(self, stages, debug=False, strict_same=True):
        self.stages = stages
        nc = bass.Bass("TRN2", target_bir_lowering=False)
        self.nc = nc
        self.S = Sched(nc, strict_same=strict_same)
        self.es = ExitStack()
        dt = lambda n, shp, kind="ExternalInput", d=F32: nc.dram_tensor(n, shp, d, kind=kind).ap()
        self.x_in = dt("x", [SEQ, D])
        self.out = dt("out", [SEQ, D], kind="ExternalOutput")
        self.w = {}
        self.w["moba_w_qkv"] = dt("moba_w_qkv", [2, D, 3 * D])
        self.w["moba_w_o"] = dt("moba_w_o", [2, D, D])
        self.w["gmlp_w_in"] = dt("gmlp_w_in", [2, D, 2 * DV])
        self.w["gmlp_b_in"] = dt("gmlp_b_in", [2, 2 * DV])
        self.w["gmlp_ln_g"] = dt("gmlp_ln_g", [2, DV])
        self.w["gmlp_ln_b"] = dt("gmlp_ln_b", [2, DV])
        self.w["gmlp_w_sT"] = dt("gmlp_w_sT", [2, 128, 8, 128])
        self.w["gmlp_b_sT"] = dt("gmlp_b_sT", [2, 128, 8])
        self.w["gmlp_w_out"] = dt("gmlp_w_out", [2, DV, D])
        for n in ("ln1_g", "ln1_b", "ln2_g", "ln2_b"):
            self.w[n] = dt(n, [DEPTH, D])
        self.w["moe_w_r"] = dt("moe_w_r", [DEPTH, D, 72])
        self.w["moe_b_r"] = dt("moe_b_r", [DEPTH, 72])
        self.w["moe_w1"] = dt("moe_w1", [DEPTH, 64, D, 256])
        self.w["moe_w3"] = dt("moe_w3", [DEPTH, 64, D, 256])
        self.w["moe_w2"] = dt("moe_w2", [DEPTH, 64, 256, D])
        self.c = {}
        self.c["ident_f"] = dt("c_ident_f", [128, 128])
        self.c["tri"] = dt("c_tri", [128, 128])
        self.c["ustr"] = dt("c_ustr", [128, 128])
        self.c["blkind"] = dt("c_blkind", [8, SEQ])
        self.c["slotbase"] = dt("c_slotbase", [128, 64])
        self.xg_d = dt("xg_scratch", [NSLOT, D], kind="Internal", d=BF16)
        self.y_d = dt("y_scratch", [NSLOT, D], kind="Internal", d=BF16)

    def sb(self, es, name, shape, dtype):
        self._uid = getattr(self, "_uid", 0) + 1
        return es.enter_context(self.nc.sbuf_tensor(f"sb{self._uid}_{name}", shape, dtype))

    def build(self):
        nc, S = self.nc, self.S
        es = self.es
        self.x = self.sb(es, "x", [128, NT, D], F32)
        self.xT = self.sb(es, "xT", [128, DC, SEQ], BF16)
        self.psA = es.enter_context(nc.psum_tensor("psA", [128, 8, 512], F32))
        self.psB = self.psA[:].bitcast(BF16)
        self.ident_f = self.sb(es, "ident_f", [128, 128], F32)
        self.ident_b = self.sb(es, "ident_b", [128, 128], BF16)
        self.tri_b = self.sb(es, "tri_b", [128, 128], BF16)
        self.ustr_b = self.sb(es, "ustr_b", [128, 128], BF16)
        self.ones_b = self.sb(es, "ones_b", [128, 128], BF16)
        self.slotbase = self.sb(es, "slotbase", [128, 64], F32)
        self.eps_t = self.sb(es, "eps_t", [128, 1], F32)
        self.zero_b = self.sb(es, "zero_b", [128, D], BF16)
        self.mhalf = self.sb(es, "mhalf", [128, NT], F32)
        self.lng = self.sb(es, "lng", [128, D], F32)
        self.lnb = self.sb(es, "lnb", [128, D], F32)
        self.lnst = self.sb(es, "lnst", [128, NT, 2, 6], F32)
        self.lnmv = self.sb(es, "lnmv", [128, NT, 4], F32)

        S.dma("sp", lambda: nc.sync.dma_start(out=self.ident_f[:], in_=self.c["ident_f"]), writes=["ident_f"])
        S.dma("pool", lambda: nc.gpsimd.dma_start(out=self.ident_b[:], in_=self.c["ident_f"]), writes=["ident_b"])
        S.dma("pool", lambda: nc.gpsimd.dma_start(out=self.tri_b[:], in_=self.c["tri"]), writes=["tri_b"])
        S.dma("pool", lambda: nc.gpsimd.dma_start(out=self.ustr_b[:], in_=self.c["ustr"]), writes=["ustr_b"])
        S.dma("sp", lambda: nc.sync.dma_start(out=self.slotbase[:], in_=self.c["slotbase"]), writes=["slotbase"])
        S.op("dve", lambda: nc.vector.memset(self.ones_b[:], 1.0), writes=["ones_b"])
        S.op("dve", lambda: nc.vector.memset(self.eps_t[:], EPS), writes=["eps_t"])
        S.op("dve", lambda: nc.vector.memset(self.zero_b[:], 0.0), writes=["zero_b"])
        S.op("dve", lambda: nc.vector.memset(self.mhalf[:], -0.5), writes=["mhalf"])
        for t in range(NT):
            S.dma("sp", lambda t=t: nc.sync.dma_start(out=self.x[:, t, :], in_=self.x_in[t * 128:(t + 1) * 128, :]),
                  writes=[("x", t)])
        self.need_zero_init = any(k == "moe" for k, _ in self.stages)
        if self.need_zero_init and self.stages[0][0] == "moe":
            self.emit_zero_init([])
        first = True
        for st in self.stages:
            kind, i = st
            if kind in ("moba", "gmlp"):
                if first:
                    for t in range(NT):
                        self.make_xT(t)
                if kind == "moba":
                    self.moba_phase(i)
                else:
                    self.gmlp_phase(i)
            else:
                idx = self.stages.index(st)
                self.moe_phase(i, make_xT_after=(idx + 1 < len(self.stages)))
            first = False
        for t in range(NT):
            S.dma("sp", lambda t=t: nc.sync.dma_start(out=self.out[t * 128:(t + 1) * 128, :], in_=self.x[:, t, :]),
                  reads=[("x", t)])
        S.finish("sp")
        return nc

    def emit_zero_init(self, after_keys):
        nc, S = self.nc, self.S
        if not self.need_zero_init:
            return
        self.need_zero_init = False
        zt = self.zero_b
        for q in range(4):
            S.dma("sp", lambda q=q: nc.sync.dma_start(
                out=self.xg_d[q * 2048:(q + 1) * 2048, :].rearrange("(e p) d -> p e d", p=128),
                in_=zt[:, :].unsqueeze(1).to_broadcast([128, 16, D])),
                reads=["zero_b"] + list(after_keys), writes=[("xgd_init", q)])

    def bank(self, b):
        return self.psA[:, b, :]

    def make_xT(self, t, banks=(6, 7)):
        nc, S = self.nc, self.S
        for hb in range(2):
            b = banks[hb]
            S.op("pe", [lambda c=c, b=b: nc.tensor.transpose(out=self.psA[:, b, (c % 4) * 128:(c % 4 + 1) * 128],
                                                              in_=self.x[:, t, c * 128:(c + 1) * 128],
                                                              identity=self.ident_f[:])
                        for c in range(hb * 4, hb * 4 + 4)],
                 reads=[("x", t), "ident_f"], writes=[("ps", b)])
            S.op("act", lambda b=b, hb=hb: nc.scalar.copy(out=self.xT[:, hb * 4:hb * 4 + 4, t * 128:(t + 1) * 128],
                                                           in_=self.psA[:, b, :].rearrange("p (c n) -> p c n", c=4)),
                 reads=[("ps", b)], writes=[("xT", t)])

    def load_ln(self, gname, bname, i):
        nc, S = self.nc, self.S
        S.dma("sp", lambda: nc.sync.dma_start(out=self.lng[:], in_=self.w[gname][i:i + 1, :].partition_broadcast(128)),
              writes=["lng"])
        S.dma("sp", lambda: nc.sync.dma_start(out=self.lnb[:], in_=self.w[bname][i:i + 1, :].partition_broadcast(128)),
              writes=["lnb"])

    def layer_norm_tiles(self, tiles):
        nc, S = self.nc, self.S
        st, mv = self.lnst, self.lnmv
        t0, n = tiles[0], len(tiles)
        assert tiles == list(range(t0, t0 + n))
        for t in tiles:
            S.op("dve", [lambda h=h, t=t: nc.vector.bn_stats(out=st[:, t, h, :], in_=self.x[:, t, h * 512:(h + 1) * 512])
                         for h in range(2)], reads=[("x", t)], writes=[("lnst", t)])
        for t in tiles:
            S.op("dve", lambda t=t: nc.vector.bn_aggr(out=mv[:, t, 0:2], in_=st[:, t, :, :].rearrange("p a b -> p (a b)")),
                 reads=[("lnst", t)], writes=[("lnmv01", t)])
        k01 = [("lnmv01", t) for t in tiles]
        k2 = [("lnmv2", t) for t in tiles]
        k3 = [("lnmv3", t) for t in tiles]
        S.op("pool", lambda: nc.gpsimd.tensor_scalar(out=mv[:, t0:t0 + n, 2], in0=mv[:, t0:t0 + n, 1], scalar1=EPS, scalar2=None,
                                                     op0=ALU.add), reads=k01, writes=k2)
        S.op("pool", lambda: nc.gpsimd.tensor_tensor(out=mv[:, t0:t0 + n, 2], in0=mv[:, t0:t0 + n, 2], in1=self.mhalf[:, 0:n],
                                                     op=ALU.pow), reads=k2 + ["mhalf"], writes=k2)
        S.op("dve", lambda: nc.vector.scalar_tensor_tensor(out=mv[:, t0:t0 + n, 3], in0=mv[:, t0:t0 + n, 0], scalar=-1.0,
                                                           in1=mv[:, t0:t0 + n, 2], op0=ALU.mult, op1=ALU.mult),
             reads=k01 + k2, writes=k3)
        for t in tiles:
            S.op("act", lambda t=t: nc.scalar.activation(out=self.x[:, t, :], in_=self.x[:, t, :], func=AF.Identity,
                                                         bias=mv[:, t, 3:4], scale=mv[:, t, 2:3]),
                 reads=[("x", t), ("lnmv2", t), ("lnmv3", t)], writes=[("x", t)])
        for t in tiles:
            S.op("dve", lambda t=t: nc.vector.tensor_tensor(out=self.x[:, t, :], in0=self.x[:, t, :], in1=self.lng[:], op=ALU.mult),
                 reads=[("x", t), "lng"], writes=[("x", t)])
        for t in tiles:
            S.op("dve", lambda t=t: nc.vector.tensor_tensor(out=self.x[:, t, :], in0=self.x[:, t, :], in1=self.lnb[:], op=ALU.add),
                 reads=[("x", t), "lnb"], writes=[("x", t)])

    def layer_norm_tile(self, t):
        self.layer_norm_tiles([t])

    def scale_x(self, t):
        nc, S = self.nc, self.S
        S.op("act", lambda: nc.scalar.mul(out=self.x[:, t, :], in_=self.x[:, t, :], mul=ALPHA),
             reads=[("x", t)], writes=[("x", t)])

    def gmlp_phase(self, i):
        nc, S = self.nc, self.S
        j = i // 2
        W = self.w
        TS = 8
        S.barrier()
        with ExitStack() as es:
            v = self.sb(es, "g_v", [128, TS, DV], BF16)
            wv = [self.sb(es, f"g_wv{k}", [128, 8, 384], BF16) for k in range(2)]
            bv = [self.sb(es, f"g_bv{k}", [128, 384], F32) for k in range(2)]
            wo = [self.sb(es, f"g_wo{k}", [128, 3, D], BF16) for k in range(2)]
            tmp = [self.sb(es, f"g_tmp{k}", [128, 384], F32) for k in range(2)]
            stats = self.sb(es, "g_stats", [128, TS, 8, 6], F32)
            mv = self.sb(es, "g_mv", [128, TS, 4], F32)
            lvg = self.sb(es, "g_lvg", [128, DV], BF16)
            lvb = self.sb(es, "g_lvb", [128, DV], BF16)
            wsT = self.sb(es, "g_wsT", [128, 8, 128], BF16)
            bsT = self.sb(es, "g_bsT", [128, 8], F32)
            u = [self.sb(es, f"g_u{k}", [128, 384], BF16) for k in range(2)]
            gate = [self.sb(es, f"g_gate{k}", [128, 384], BF16) for k in range(2)]
            gateT = [self.sb(es, f"g_gateT{k}", [128, 3, 128], BF16) for k in range(2)]

            self.load_ln("ln1_g", "ln1_b", i)
            S.dma("pool", lambda: nc.gpsimd.dma_start(out=lvg[:], in_=W["gmlp_ln_g"][j:j + 1, :].partition_broadcast(128)),
                  writes=["lvg"])
            S.dma("pool", lambda: nc.gpsimd.dma_start(out=lvb[:], in_=W["gmlp_ln_b"][j:j + 1, :].partition_broadcast(128)),
                  writes=["lvb"])
            S.dma("pool", lambda: nc.gpsimd.dma_start(out=wsT[:], in_=W["gmlp_w_sT"][j]), writes=["wsT"])
            S.dma("sp", lambda: nc.sync.dma_start(out=bsT[:], in_=W["gmlp_b_sT"][j]), writes=["bsT"])
            for g in range(8):
                S.op("dve", lambda g=g: nc.vector.tensor_tensor(out=wsT[:, g, :], in0=wsT[:, g, :], in1=self.tri_b[:],
                                                                op=ALU.mult),
                     reads=["wsT", "tri_b"], writes=["wsT"])
            for t in range(NT):
                self.scale_x(t)

            cnt = 0
            for st in range(NT // TS):
                for k in range(8):
                    wb = k % 2
                    c0 = DV + k * 384
                    S.dma("pool", lambda wb=wb, c0=c0: nc.gpsimd.dma_start(
                        out=wv[wb][:], in_=W["gmlp_w_in"][j, :, c0:c0 + 384].rearrange("(c p) f -> p c f", p=128)),
                        writes=[("wv", wb)])
                    S.dma("sp", lambda wb=wb, c0=c0: nc.sync.dma_start(
                        out=bv[wb][:], in_=W["gmlp_b_in"][j:j + 1, c0:c0 + 384].partition_broadcast(128)),
                        writes=[("bv", wb)])
                    for tt in range(TS):
                        t = st * TS + tt
                        b = cnt % 4
                        tb = cnt % 2
                        cnt += 1
                        S.op("pe", [lambda c=c, b=b, t=t, wb=wb: nc.tensor.matmul(
                            self.psA[:, b, 0:384], lhsT=self.xT[:, c, t * 128:(t + 1) * 128], rhs=wv[wb][:, c, :],
                            start=(c == 0), stop=(c == 7)) for c in range(8)],
                            reads=[("xT", t), ("wv", wb)], writes=[("ps", b)])
                        S.op("dve", lambda b=b, tb=tb, wb=wb: nc.vector.tensor_tensor(
                            out=tmp[tb][:], in0=self.psA[:, b, 0:384], in1=bv[wb][:], op=ALU.add),
                            reads=[("ps", b), ("bv", wb)], writes=[("tmp", tb)])
                        S.op("act", lambda tb=tb, tt=tt, k=k: nc.scalar.activation(
                            out=v[:, tt, k * 384:(k + 1) * 384], in_=tmp[tb][:], func=AF.Gelu),
                            reads=[("tmp", tb)], writes=[("v", tt, k)])
                        S.op("dve", lambda tt=tt, k=k: nc.vector.bn_stats(
                            out=stats[:, tt, k, :], in_=v[:, tt, k * 384:(k + 1) * 384]),
                            reads=[("v", tt, k)], writes=[("stats", tt, k)])
                if st == 0:
                    self.emit_zero_init([("v", 0, 0)])
                for tt in range(TS):
                    S.op("dve", lambda tt=tt: nc.vector.bn_aggr(out=mv[:, tt, 0:2],
                                                                in_=stats[:, tt, :, :].rearrange("p a b -> p (a b)")),
                         reads=[("stats", tt, k) for k in range(8)], writes=[("mv", tt)])
                kmv = [("mv", tt) for tt in range(TS)]
                S.op("pool", lambda: nc.gpsimd.tensor_scalar(out=mv[:, :, 2], in0=mv[:, :, 1], scalar1=EPS, scalar2=None, op0=ALU.add),
                     reads=kmv, writes=["mv2"])
                S.op("pool", lambda: nc.gpsimd.tensor_tensor(out=mv[:, :, 2], in0=mv[:, :, 2], in1=self.mhalf[:, 0:TS], op=ALU.pow),
                     reads=["mv2", "mhalf"], writes=["mv2"])
                S.op("dve", lambda: nc.vector.scalar_tensor_tensor(out=mv[:, :, 3], in0=mv[:, :, 0], scalar=-1.0, in1=mv[:, :, 2],
                                                                   op0=ALU.mult, op1=ALU.mult), reads=kmv + ["mv2"], writes=["mv3"])
                for tt in range(TS):
                    vk = [("v", tt, k) for k in range(8)]
                    S.op("act", lambda tt=tt: nc.scalar.activation(out=v[:, tt, :], in_=v[:, tt, :], func=AF.Identity,
                                                                   bias=mv[:, tt, 3:4], scale=mv[:, tt, 2:3]),
                         reads=vk + ["mv2", "mv3"], writes=vk)
                for tt in range(TS):
                    vk = [("v", tt, k) for k in range(8)]
                    S.op("dve", lambda tt=tt: nc.vector.tensor_tensor(out=v[:, tt, :], in0=v[:, tt, :], in1=lvg[:],
                                                                      op=ALU.mult), reads=vk + ["lvg"], writes=vk)
                    S.op("dve", lambda tt=tt: nc.vector.tensor_tensor(out=v[:, tt, :], in0=v[:, tt, :], in1=lvb[:],
                                                                      op=ALU.add), reads=vk + ["lvb"], writes=vk)
                def load_wv(g):
                    wb = g % 2
                    c0 = g * 384
                    S.dma("pool", lambda: nc.gpsimd.dma_start(
                        out=wv[wb][:], in_=W["gmlp_w_in"][j, :, c0:c0 + 384].rearrange("(c p) f -> p c f", p=128)),
                        writes=[("wv", wb)])
                    S.dma("sp", lambda: nc.sync.dma_start(
                        out=bv[wb][:], in_=W["gmlp_b_in"][j:j + 1, c0:c0 + 384].partition_broadcast(128)),
                        writes=[("bv", wb)])

                def load_wo(g):
                    wb = g % 2
                    c0 = g * 384
                    S.dma("pool", lambda: nc.gpsimd.dma_start(
                        out=wo[wb][:], in_=W["gmlp_w_out"][j, c0:c0 + 384, :].rearrange("(c p) f -> p c f", p=128)),
                        writes=[("wo", wb)])

                def p2_A(n):
                    g, tt = n // TS, n % TS
                    t = st * TS + tt
                    wb = g % 2
                    ub = n % 2
                    bu = ub
                    bm = 2 + ub
                    c0 = g * 384
                    S.op("pe", [lambda c=c: nc.tensor.matmul(
                        self.psA[:, bu, 0:384], lhsT=self.xT[:, c, t * 128:(t + 1) * 128], rhs=wv[wb][:, c, :],
                        start=(c == 0), stop=(c == 7)) for c in range(8)],
                        reads=[("xT", t), ("wv", wb)], writes=[("ps", bu)])
                    S.op("dve", lambda: nc.vector.tensor_tensor(
                        out=tmp[ub][:], in0=self.psA[:, bu, 0:384], in1=bv[wb][:], op=ALU.add),
                        reads=[("ps", bu), ("bv", wb)], writes=[("tmp", ub)])
                    S.op("act", lambda: nc.scalar.activation(out=u[ub][:], in_=tmp[ub][:], func=AF.Gelu),
                         reads=[("tmp", ub)], writes=[("u", ub)])
                    S.op("pe", lambda: nc.tensor.matmul(
                        self.psA[:, bm, 0:384], lhsT=wsT[:, g, :], rhs=v[:, tt, c0:c0 + 384], start=True, stop=True),
                        reads=["wsT", ("v", tt, g)], writes=[("ps", bm)])
                    if tt == TS - 1 and g + 2 < 8:
                        load_wv(g + 2)

                def p2_B(n):
                    g, tt = n // TS, n % TS
                    ub = n % 2
                    bm = 2 + ub
                    bt = 4
                    S.op("dve", lambda: nc.vector.scalar_tensor_tensor(
                        out=gate[ub][:], in0=self.psA[:, bm, 0:384], scalar=bsT[:, g:g + 1], in1=u[ub][:],
                        op0=ALU.add, op1=ALU.mult),
                        reads=[("ps", bm), "bsT", ("u", ub)], writes=[("gate", ub)])

                def p2_Bt(n):
                    ub = n % 2
                    bt = 4
                    S.op("pe", [lambda q=q: nc.tensor.transpose(
                        out=self.psB[:, bt, q * 128:(q + 1) * 128], in_=gate[ub][:, q * 128:(q + 1) * 128],
                        identity=self.ident_b[:]) for q in range(3)],
                        reads=[("gate", ub), "ident_b"], writes=[("ps", bt)])
                    S.op("act", lambda: nc.scalar.copy(
                        out=gateT[ub][:], in_=self.psB[:, bt, 0:384].rearrange("p (c n) -> p c n", c=3)),
                        reads=[("ps", bt)], writes=[("gateT", ub)])

                def p2_C(n):
                    g, tt = n // TS, n % TS
                    t = st * TS + tt
                    wb = g % 2
                    ub = n % 2
                    bh = 5
                    S.op("pe", [lambda q=q, hf=hf: nc.tensor.matmul(
                        self.psA[:, bh + hf, :], lhsT=gateT[ub][:, q, :], rhs=wo[wb][:, q, hf * 512:(hf + 1) * 512],
                        start=(q == 0), stop=(q == 2)) for hf in range(2) for q in range(3)],
                        reads=[("gateT", ub), ("wo", wb)], writes=[("ps", bh), ("ps", bh + 1)])
                    S.op("dve", lambda: nc.vector.tensor_tensor(
                        out=self.x[:, t, :], in0=self.psA[:, bh:bh + 2, :].rearrange("p a n -> p (a n)"),
                        in1=self.x[:, t, :], op=ALU.add),
                        reads=[("ps", bh), ("ps", bh + 1), ("x", t)], writes=[("x", t)])
                    if tt == TS - 1 and g + 2 < 8:
                        load_wo(g + 2)

                for g in range(2):
                    load_wv(g)
                    load_wo(g)
                NST = 8 * TS
                for n in range(NST + 2):
                    if 0 <= n - 1 < NST:
                        p2_B(n - 1)
                    if n < NST:
                        p2_A(n)
                    if 0 <= n - 2 < NST:
                        p2_C(n - 2)
                    if 0 <= n - 1 < NST:
                        p2_Bt(n - 1)
            for t0 in range(0, NT, 4):
                self.layer_norm_tiles(list(range(t0, t0 + 4)))
            S.barrier()

    def moba_phase(self, i):
        nc, S = self.nc, self.S
        W = self.w
        j = i // 2
        S.barrier()
        with ExitStack() as es:
            OT = self.sb(es, "a_OT", [128, 8, SEQ], BF16)
            QA = [self.sb(es, f"a_QA{h}", [128, SEQ], BF16) for h in range(2)]
            KA = [self.sb(es, f"a_KA{h}", [128, SEQ], BF16) for h in range(2)]
            VA = [self.sb(es, f"a_VA{h}", [128, NT, 128], BF16) for h in range(2)]
            Wq = [self.sb(es, f"a_Wq{k}", [128, 8, 128], BF16) for k in range(2)]
            Wk = [self.sb(es, f"a_Wk{k}", [128, 8, 128], BF16) for k in range(2)]
            Wv = [self.sb(es, f"a_Wv{k}", [128, 8, 128], BF16) for k in range(2)]
            Wo = self.sb(es, "a_Wo", [128, 8, D], BF16)
            NPT = 5
            PT = [self.sb(es, f"a_PT{k}", [128, 512], BF16) for k in range(NPT)]
            rden = [self.sb(es, f"a_rden{k}", [128, 512], F32) for k in range(2)]
            kms = [self.sb(es, f"a_kms{h}", [64, 8], F32) for h in range(2)]
            kmb = [self.sb(es, f"a_kmb{h}", [128, 8], BF16) for h in range(2)]
            bsw = self.sb(es, "a_bsw", [128, 16, 8], F32)
            bsw2 = self.sb(es, "a_bsw2", [128, 16, 8], F32)
            beq = self.sb(es, "a_beq", [128, 16, 8], F32)
            bmx = self.sb(es, "a_bmx", [128, 16], F32)
            bpw = self.sb(es, "a_bpw", [128, 16, 72], BF16)

            for h in range(2):
                S.op("dve", lambda h=h: nc.vector.memset(QA[h][64:128, :], 0.0), writes=[("QApad", h)])
                S.op("dve", lambda h=h: nc.vector.memset(KA[h][64:128, :], 0.0), writes=[("KApad", h)])
                S.op("dve", lambda h=h: nc.vector.memset(kmb[h][:], 0.0), writes=[("kmb", h)])
            self.load_ln("ln1_g", "ln1_b", i)
            S.dma("pool", lambda: nc.gpsimd.dma_start(out=Wo[:], in_=W["moba_w_o"][j].rearrange("(c p) f -> p c f", p=128)),
                  writes=["Wo"])
            for h in range(2):
                if "noblk" in DBG:
                    continue
                for hh in range(2):
                    S.dma("pool", lambda h=h, hh=hh: nc.gpsimd.dma_start(out=KA[h][64:72, hh * 1024:(hh + 1) * 1024],
                                                                        in_=self.c["blkind"][:, hh * 1024:(hh + 1) * 1024]),
                          reads=[("KApad", h)], writes=[("KAb", h, hh)])

            S.op("dve", lambda: nc.vector.memset(bpw[:], 0.0), writes=["bpw"])
            S.op("dve", lambda: nc.vector.memset(VA[0][:, :, 64:128], 1.0), writes=[("VAones", 0)])
            S.op("dve", lambda: nc.vector.memset(VA[1][:, :, 0:64], 1.0), writes=[("VAones", 1)])
            for t in range(NT):
                self.scale_x(t)

            def load_pair_w(p):
                b = p % 2
                for nm, dst, off in (("Wq", Wq, 0), ("Wk", Wk, D), ("Wv", Wv, 2 * D)):
                    S.dma("pool", lambda dst=dst, off=off: nc.gpsimd.dma_start(
                        out=dst[b][:], in_=W["moba_w_qkv"][j, :, off + p * 128: off + (p + 1) * 128].rearrange("(c p) f -> p c f", p=128)),
                        writes=[(nm, b)])

            if STOP == 1:
                return
            load_pair_w(0)
            bpc = 0
            for p in range(8):
                wb = p % 2
                if p + 1 < 8:
                    load_pair_w(p + 1)
                if STOP == 5:
                    return
                for tq in range(4):
                    cols = slice(tq * 512, (tq + 1) * 512)
                    xk = [("xT", t) for t in range(tq * 4, tq * 4 + 4)]
                    bq = 5
                    S.op("pe", [lambda c=c: nc.tensor.matmul(self.psA[:, bq, :], lhsT=Wq[wb][:, c, :], rhs=self.xT[:, c, cols],
                                                             start=(c == 0), stop=(c == 7)) for c in range(8)],
                         reads=xk + [("Wq", wb)], writes=[("ps", bq)])
                    for h in range(2):
                        S.op("act", lambda h=h: nc.scalar.mul(out=QA[h][0:64, cols], in_=self.psA[h * 64:(h + 1) * 64, bq, :], mul=0.125),
                             reads=[("ps", bq)], writes=[("QA", h, t) for t in range(tq * 4, tq * 4 + 4)])
                    if STOP == 6:
                        return
                    bk = 6
                    S.op("pe", [lambda c=c: nc.tensor.matmul(self.psA[:, bk, :], lhsT=Wk[wb][:, c, :], rhs=self.xT[:, c, cols],
                                                             start=(c == 0), stop=(c == 7)) for c in range(8)],
                         reads=xk + [("Wk", wb)], writes=[("ps", bk)])
                    for h in range(2):
                        S.op("act", lambda h=h: nc.scalar.copy(out=KA[h][0:64, cols], in_=self.psA[h * 64:(h + 1) * 64, bk, :]),
                             reads=[("ps", bk)], writes=[("KA", h, t) for t in range(tq * 4, tq * 4 + 4)])
                    for h in range(2):
                        S.op("dve", lambda h=h: nc.vector.tensor_reduce(
                            out=kms[h][:, 2 * tq:2 * tq + 2],
                            in_=self.psA[h * 64:(h + 1) * 64, bk, :].rearrange("p (a n) -> p a n", a=2), axis=AX.X, op=ALU.add),
                            reads=[("ps", bk), ("KA", 0, tq * 4), ("KA", 1, tq * 4)], writes=[("kms", h)])
                if STOP == 2:
                    return
                for h in range(2):
                    S.op("dve", lambda h=h: nc.vector.tensor_copy(out=kmb[h][0:64, :], in_=kms[h][:]),
                         reads=[("kms", h)], writes=[("kmb", h)])
                for g4 in range(4):
                    bv_ = 7
                    S.op("pe", [lambda c=c, tl=tl: nc.tensor.matmul(
                        self.psA[:, bv_, tl * 128:(tl + 1) * 128], lhsT=self.xT[:, c, (g4 * 4 + tl) * 128:(g4 * 4 + tl + 1) * 128],
                        rhs=Wv[wb][:, c, :], start=(c == 0), stop=(c == 7)) for tl in range(4) for c in range(8)],
                        reads=[("xT", t) for t in range(g4 * 4, g4 * 4 + 4)] + [("Wv", wb)], writes=[("ps", bv_)])
                    pv = self.psA[:, bv_, :].rearrange("p (a n) -> p a n", a=4)
                    S.op("act", lambda: nc.scalar.copy(out=VA[0][:, g4 * 4:g4 * 4 + 4, 0:64], in_=pv[:, :, 0:64]),
                         reads=[("ps", bv_)], writes=[("VA", 0, t) for t in range(g4 * 4, g4 * 4 + 4)])
                    S.op("act", lambda: nc.scalar.copy(out=VA[1][:, g4 * 4:g4 * 4 + 4, 64:128], in_=pv[:, :, 64:128]),
                         reads=[("ps", bv_)], writes=[("VA", 1, t) for t in range(g4 * 4, g4 * 4 + 4)])
                if STOP == 3:
                    return
                if "nobias" not in DBG:
                    S.op("pe", [lambda h=h, qt=qt: nc.tensor.matmul(
                        self.psA[:, 7, (qt - 8) * 16 + h * 8:(qt - 8) * 16 + (h + 1) * 8], lhsT=QA[h][:, qt * 128:(qt + 1) * 128],
                        rhs=kmb[h][:, :], start=True, stop=True) for qt in range(8, 16) for h in range(2)],
                        reads=[("QA", h, qt) for h in range(2) for qt in range(8, 16)] +
                              [("kmb", 0), ("kmb", 1), ("QApad", 0), ("QApad", 1)] +
                              [("QAb", h, qt) for h in range(2) for qt in range(8, 16)], writes=[("ps", 7)])
                    S.op("dve", lambda: nc.vector.memset(bsw[:], -1.0e30), writes=["bsw"])
                    psv = self.psA[:, 7, 0:128].rearrange("p (q h e) -> p q h e", q=8, h=2)
                    for cur in range(4, 8):
                        q0 = 2 * (cur - 4)
                        S.op("dve", lambda cur=cur, q0=q0: nc.vector.tensor_copy(
                            out=bsw[:].rearrange("p (q h) e -> p q h e", h=2)[:, q0:q0 + 2, :, 0:cur], in_=psv[:, q0:q0 + 2, :, 0:cur]),
                            reads=[("ps", 7)], writes=["bsw"])
                    S.op("dve", lambda: nc.vector.tensor_copy(out=bsw2[:], in_=bsw[:]), reads=["bsw"], writes=["bsw2"])
                    for rnd in range(2):
                        S.op("dve", lambda: nc.vector.tensor_reduce(out=bmx[:], in_=bsw2[:], axis=AX.X, op=ALU.max),
                             reads=["bsw2"], writes=["bmx"])
                        S.op("dve", lambda: nc.vector.tensor_tensor(out=beq[:], in0=bsw2[:], in1=bmx[:].unsqueeze(2).to_broadcast([128, 16, 8]),
                                                                    op=ALU.is_equal), reads=["bsw2", "bmx"], writes=["beq"])
                        S.op("dve", lambda: nc.vector.scalar_tensor_tensor(
                            out=bsw2[:].rearrange("p g e -> p (g e)"), in0=beq[:].rearrange("p g e -> p (g e)"), scalar=-3.0e30,
                            in1=bsw2[:].rearrange("p g e -> p (g e)"), op0=ALU.mult, op1=ALU.add),
                            reads=["beq", "bsw2"], writes=["bsw2"])
                    S.op("dve", lambda: nc.vector.tensor_reduce(out=bmx[:], in_=bsw2[:], axis=AX.X, op=ALU.max),
                         reads=["bsw2"], writes=["bmx"])
                    S.op("dve", lambda: nc.vector.tensor_tensor(out=beq[:], in0=bsw[:], in1=bmx[:].unsqueeze(2).to_broadcast([128, 16, 8]),
                                                                op=ALU.is_lt), reads=["bsw", "bmx"], writes=["beq"])
                    S.op("dve", lambda: nc.vector.tensor_scalar(out=bpw[:, :, 64:72], in0=beq[:], scalar1=NEG, scalar2=None, op0=ALU.mult),
                         reads=["beq"], writes=["bpw"])
                    for cur in range(4, 8):
                        g0 = 4 * (cur - 4)
                        S.op("dve", lambda cur=cur, g0=g0: nc.vector.memset(bpw[:, g0:g0 + 4, 64 + cur:65 + cur], 0.0),
                             reads=["bpw"], writes=["bpw"])
                    bbanks = {0: (5, 6), 1: (7, 0)}
                    for h in range(2):
                        for half in range(2):
                            bb = bbanks[h][half]
                            S.op("pe", [lambda h=h, qq=qq, bb=bb, half=half: nc.tensor.matmul(
                                self.psA[0:72, bb, qq * 128:(qq + 1) * 128], lhsT=bpw[:, (half * 4 + qq) * 2 + h, 0:72],
                                rhs=self.ident_b[:], start=True, stop=True) for qq in range(4)],
                                reads=["bpw", "ident_b"], writes=[("ps", bb)])
                            S.op("act", lambda h=h, bb=bb, half=half: nc.scalar.copy(
                                out=QA[h][64:72, 1024 + half * 512:1024 + (half + 1) * 512], in_=self.psA[64:72, bb, :]),
                                reads=[("ps", bb)], writes=[("QAb", h, qt) for qt in range(8 + half * 4, 12 + half * 4)])
                if STOP == 4:
                    return
                items = []
                for h in range(2):
                    for Qc in range(4):
                        nkt = 4 * Qc + 4
                        for kt in range(nkt):
                            items.append((h, Qc, kt, nkt))
                state = {"n": 0}

                def emit_S(n):
                    h, Qc, kt, nkt = items[n]
                    ii = kt - 4 * Qc
                    c0 = max(0, ii) * 128
                    sbk = n % 3
                    pb = n % NPT
                    Kd = 128
                    qt0 = 4 * Qc + c0 // 128
                    rd = [("KA", h, kt), ("QApad", h), ("KApad", h), ("KAb", h, 0), ("KAb", h, 1)]
                    rd += [("QA", h, t) for t in range(qt0, 4 * Qc + 4)]
                    if Qc >= 2:
                        rd += [("QAb", h, t) for t in range(qt0, 4 * Qc + 4)]
                    S.op("pe", lambda: nc.tensor.matmul(self.psA[:, sbk, c0:512], lhsT=KA[h][0:Kd, kt * 128:(kt + 1) * 128],
                                                        rhs=QA[h][0:Kd, Qc * 512 + c0:(Qc + 1) * 512], start=True, stop=True),
                         reads=rd, writes=[("ps", sbk)])
                    S.op("act", lambda: nc.scalar.activation(out=PT[pb][:, c0:512], in_=self.psA[:, sbk, c0:512], func=AF.Exp),
                         reads=[("ps", sbk)], writes=[("PT", pb)])
                    if ii >= 0:
                        S.op("pool", lambda: nc.gpsimd.tensor_tensor(out=PT[pb][:, c0:c0 + 128], in0=PT[pb][:, c0:c0 + 128],
                                                                      in1=self.tri_b[:], op=ALU.mult),
                             reads=[("PT", pb), "tri_b"], writes=[("PT", pb)])

                def emit_PV(n):
                    h, Qc, kt, nkt = items[n]
                    ii = kt - 4 * Qc
                    c0 = max(0, ii) * 128
                    pb = n % NPT
                    ob = 3 + ((h * 4 + Qc) % 3)
                    S.op("pe", lambda: nc.tensor.matmul(self.psA[:, ob, c0:512], lhsT=VA[h][:, kt, :], rhs=PT[pb][:, c0:512],
                                                        start=(kt == 0), stop=(kt == nkt - 1)),
                         reads=[("PT", pb), ("VA", h, kt), ("VAones", h)], writes=[("ps", ob)])
                    if kt == nkt - 1:
                        r = (h * 4 + Qc) % 2
                        osl = slice(h * 64, (h + 1) * 64)
                        dsl = slice((1 - h) * 64, (2 - h) * 64)
                        S.op("dve", lambda: nc.vector.reciprocal(out=rden[r][osl, :], in_=self.psA[dsl, ob, :]),
                             reads=[("ps", ob)], writes=[("rden", r)])
                        S.op("dve", lambda: nc.vector.tensor_tensor(out=OT[osl, p, Qc * 512:(Qc + 1) * 512], in0=self.psA[osl, ob, :],
                                                                    in1=rden[r][osl, :], op=ALU.mult),
                             reads=[("ps", ob), ("rden", r)], writes=[("OT", p, h, Qc)])

                LA = 3
                if "noattn" in DBG:
                    items = []
                for n in range(len(items) + LA):
                    if n < len(items):
                        emit_S(n)
                    if n >= LA:
                        emit_PV(n - LA)
                if p == 0:
                    self.emit_zero_init([("OT", 0, 0, 0)])
            otk = [("OT", p, h, Qc) for p in range(8) for h in range(2) for Qc in range(4)]
            for t in range(NT):
                by = 5 + 0
                for hf in range(2):
                    S.op("pe", [lambda p=p, hf=hf: nc.tensor.matmul(self.psA[:, 5 + hf, :], lhsT=OT[:, p, t * 128:(t + 1) * 128],
                                                                    rhs=Wo[:, p, hf * 512:(hf + 1) * 512],
                                                                    start=(p == 0), stop=(p == 7)) for p in range(8)],
                         reads=[("OT", p, h, t // 4) for p in range(8) for h in range(2)] + ["Wo"], writes=[("ps", 5 + hf)])
                S.op("dve", lambda: nc.vector.tensor_tensor(out=self.x[:, t, :], in0=self.psA[:, 5:7, :].rearrange("p a n -> p (a n)"),
                                                            in1=self.x[:, t, :], op=ALU.add),
                     reads=[("ps", 5), ("ps", 6), ("x", t)], writes=[("x", t)])
                if t % 4 == 3:
                    self.layer_norm_tiles(list(range(t - 3, t + 1)))
            S.barrier()

    def moe_phase(self, i, make_xT_after=True):
        nc, S = self.nc, self.S
        W = self.w
        S.barrier()
        with ExitStack() as es:
            wr = self.sb(es, "m_wr", [128, 8, 72], F32)
            br = self.sb(es, "m_br", [128, 72], F32)
            slotf = self.sb(es, "m_slotf", [128, NT, 2], F32)
            sloti = self.sb(es, "m_sloti", [128, NT, 2], I32)
            gates = self.sb(es, "m_gates", [128, NT, 2], F32)
            NB = 4
            NBX = 4
            W13 = [self.sb(es, f"m_w13_{k}", [128, 8, 512], BF16) for k in range(NB)]
            W2 = [self.sb(es, f"m_w2_{k}", [128, 2, D], BF16) for k in range(NB)]

            self.load_ln("ln2_g", "ln2_b", i)
            S.dma("sp", lambda: nc.sync.dma_start(out=wr[:], in_=W["moe_w_r"][i].rearrange("(c p) f -> p c f", p=128)),
                  writes=["wr"])
            S.dma("sp", lambda: nc.sync.dma_start(out=br[:], in_=W["moe_b_r"][i:i + 1, :].partition_broadcast(128)),
                  writes=["br"])

            def load_w13(e):
                b = e % NB
                S.dma("pool", lambda: nc.gpsimd.dma_start(
                    out=W13[b][:, :, 0:256], in_=W["moe_w1"][i, e].rearrange("(c p) f -> p c f", p=128)),
                    writes=[("W13a", b)])
                S.dma("pool", lambda: nc.gpsimd.dma_start(
                    out=W13[b][:, :, 256:512], in_=W["moe_w3"][i, e].rearrange("(c p) f -> p c f", p=128)),
                    writes=[("W13b", b)])

            def load_w2(e):
                b = e % NB
                S.dma("pool", lambda: nc.gpsimd.dma_start(
                    out=W2[b][:], in_=W["moe_w2"][i, e].rearrange("(c p) f -> p c f", p=128)),
                    writes=[("W2", b)])

            for e in range(NB):
                load_w13(e)
                load_w2(e)

            xgd_keys = []
            with ExitStack() as rs:
                xnT = [self.sb(rs, f"m_xnT{k}", [128, 8, 128], F32) for k in range(2)]
                lg = self.sb(rs, "m_lg", [128, NT, 72], F32)
                dd = self.sb(rs, "m_dd", [128, NT, 8], F32)
                pen = self.sb(rs, "m_pen", [128, NT, 8], F32)
                ml = self.sb(rs, "m_ml", [128, NT, 64], F32)
                oh1 = self.sb(rs, "m_oh1", [128, NT, 64], F32)
                oh2 = self.sb(rs, "m_oh2", [128, NT, 64], F32)
                posb = self.sb(rs, "m_posb", [128, NT, 64], F32)
                A_b = self.sb(rs, "m_Ab", [128, NT, 64], BF16)
                sm = self.sb(rs, "m_sm", [128, 8, NT], F32)
                def rt_T(t):
                    k2 = t % 2
                    for hb_ in range(2):
                        b = hb_ if t % 2 == 0 else 6 + hb_
                        S.op("pe", [lambda c=c, b=b: nc.tensor.transpose(
                            out=self.psA[:, b, (c % 4) * 128:(c % 4 + 1) * 128], in_=self.x[:, t, c * 128:(c + 1) * 128],
                            identity=self.ident_f[:]) for c in range(hb_ * 4, hb_ * 4 + 4)],
                            reads=[("x", t), "ident_f"], writes=[("ps", b)])
                        S.op("act", lambda b=b, hb_=hb_: nc.scalar.copy(
                            out=xnT[k2][:, hb_ * 4:hb_ * 4 + 4, :], in_=self.psA[:, b, :].rearrange("p (c n) -> p c n", c=4)),
                            reads=[("ps", b)], writes=[("xnT", k2, hb_)])

                def rt_M(t):
                    k2 = t % 2
                    bl = 2 + (t // 4) % 2
                    S.op("pe", [lambda c=c: nc.tensor.matmul(self.psA[:, bl, (t % 4) * 72:(t % 4 + 1) * 72], lhsT=xnT[k2][:, c, :],
                                                             rhs=wr[:, c, :], start=(c == 0), stop=(c == 7)) for c in range(8)],
                         reads=[("xnT", k2, 0), ("xnT", k2, 1), "wr"], writes=[("ps", bl)])
                    if t % 4 == 3:
                        t0 = t - 3
                        S.op("dve", lambda: nc.vector.tensor_tensor(
                            out=lg[:, t0:t0 + 4, :], in0=self.psA[:, bl, 0:288].rearrange("p (a n) -> p a n", a=4),
                            in1=br[:, :].unsqueeze(1).to_broadcast([128, 4, 72]), op=ALU.add),
                            reads=[("ps", bl), "br"], writes=["lg"])

                for t in range(NT + 1):
                    if t < NT:
                        rt_T(t)
                    if t >= 1:
                        rt_M(t - 1)
                gmax, sume, gp, top1, top2, e2 = (sm[:, k, :] for k in range(6))
                lgg = lg[:, :, 0:8]
                S.op("dve", lambda: nc.vector.tensor_reduce(out=gmax, in_=lgg, axis=AX.X, op=ALU.max), reads=["lg"], writes=["gmax"])
                S.op("dve", lambda: nc.vector.tensor_tensor(out=dd[:], in0=lgg, in1=gmax.unsqueeze(2).to_broadcast([128, NT, 8]),
                                                            op=ALU.subtract), reads=["lg", "gmax"], writes=["dd"])
                S.op("dve", lambda: nc.vector.tensor_scalar(out=pen[:], in0=dd[:], scalar1=0.0, scalar2=-1.0e9,
                                                            op0=ALU.is_lt, op1=ALU.mult), reads=["dd"], writes=["pen"])
                S.op("act", lambda: nc.scalar.activation(out=dd[:], in_=dd[:], func=AF.Exp), reads=["dd", "pen"], writes=["dd"])
                S.op("dve", lambda: nc.vector.tensor_reduce(out=sume, in_=dd[:], axis=AX.X, op=ALU.add), reads=["dd"], writes=["sume"])
                S.op("dve", lambda: nc.vector.reciprocal(out=gp, in_=sume), reads=["sume"], writes=["gp"])
                S.op("dve", lambda: nc.vector.tensor_tensor(
                    out=ml[:].rearrange("p t (g e) -> p t g e", g=8), in0=lg[:, :, 8:72].rearrange("p t (g e) -> p t g e", g=8),
                    in1=pen[:].unsqueeze(3).to_broadcast([128, NT, 8, 8]), op=ALU.add), reads=["lg", "pen"], writes=["ml"])
                S.op("dve", lambda: nc.vector.tensor_reduce(out=top1, in_=ml[:], axis=AX.X, op=ALU.max), reads=["ml"], writes=["top1"])
                S.op("dve", lambda: nc.vector.tensor_tensor(out=oh1[:], in0=ml[:], in1=top1.unsqueeze(2).to_broadcast([128, NT, 64]),
                                                            op=ALU.is_equal), reads=["ml", "top1"], writes=["oh1"])
                S.op("dve", lambda: nc.vector.scalar_tensor_tensor(
                    out=ml[:].rearrange("p t e -> p (t e)"), in0=oh1[:].rearrange("p t e -> p (t e)"), scalar=-1.0e9,
                    in1=ml[:].rearrange("p t e -> p (t e)"), op0=ALU.mult, op1=ALU.add), reads=["oh1", "ml"], writes=["ml"])
                S.op("dve", lambda: nc.vector.tensor_reduce(out=top2, in_=ml[:], axis=AX.X, op=ALU.max), reads=["ml"], writes=["top2"])
                S.op("dve", lambda: nc.vector.tensor_tensor(out=oh2[:], in0=ml[:], in1=top2.unsqueeze(2).to_broadcast([128, NT, 64]),
                                                            op=ALU.is_equal), reads=["ml", "top2"], writes=["oh2"])
                S.op("dve", lambda: nc.vector.tensor_tensor(out=e2, in0=top2, in1=top1, op=ALU.subtract),
                     reads=["top1", "top2"], writes=["e2"])
                S.op("act", lambda: nc.scalar.activation(out=e2, in_=e2, func=AF.Exp), reads=["e2"], writes=["e2"])
                S.op("dve", lambda: nc.vector.tensor_scalar(out=e2, in0=e2, scalar1=1.0, scalar2=None, op0=ALU.add),
                     reads=["e2"], writes=["e2"])
                S.op("dve", lambda: nc.vector.reciprocal(out=e2, in_=e2), reads=["e2"], writes=["e2"])
                S.op("dve", lambda: nc.vector.tensor_tensor(out=gates[:, :, 0], in0=e2, in1=gp, op=ALU.mult),
                     reads=["e2", "gp"], writes=["gates0"])
                S.op("dve", lambda: nc.vector.tensor_tensor(out=gates[:, :, 1], in0=gp, in1=gates[:, :, 0], op=ALU.subtract),
                     reads=["gp", "gates0"], writes=["gates1"])
                S.op("dve", lambda: nc.vector.tensor_tensor(out=A_b[:], in0=oh1[:], in1=oh2[:], op=ALU.add),
                     reads=["oh1", "oh2"], writes=["A_b"])
                for t in range(NT):
                    bp = 4 + t // 8
                    cs = slice((t % 8) * 64, (t % 8 + 1) * 64)
                    mm = [lambda: nc.tensor.matmul(self.psA[:, bp, cs], lhsT=self.ustr_b[:], rhs=A_b[:, t, :], start=True, stop=(t == 0))]
                    for jj in range(t):
                        mm.append(lambda jj=jj: nc.tensor.matmul(self.psA[:, bp, cs], lhsT=self.ones_b[:], rhs=A_b[:, jj, :],
                                                                 start=False, stop=(jj == t - 1)))
                    S.op("pe", mm, reads=["A_b", "ustr_b", "ones_b"], writes=[("ps", bp)])
                S.op("dve", lambda: nc.vector.tensor_tensor(
                    out=posb[:], in0=self.psA[:, 4:6, :].rearrange("p a (t e) -> p (a t) e", e=64),
                    in1=self.slotbase[:, :].unsqueeze(1).to_broadcast([128, NT, 64]), op=ALU.add),
                    reads=[("ps", 4), ("ps", 5), "slotbase"], writes=["posb"])
                S.op("dve", lambda: nc.vector.tensor_tensor(out=oh1[:], in0=oh1[:], in1=posb[:], op=ALU.mult),
                     reads=["oh1", "posb"], writes=["oh1"])
                S.op("dve", lambda: nc.vector.tensor_reduce(out=slotf[:, :, 0], in_=oh1[:], axis=AX.X, op=ALU.add),
                     reads=["oh1"], writes=["slotf0"])
                S.op("dve", lambda: nc.vector.tensor_tensor(out=oh2[:], in0=oh2[:], in1=posb[:], op=ALU.mult),
                     reads=["oh2", "posb"], writes=["oh2"])
                S.op("dve", lambda: nc.vector.tensor_reduce(out=slotf[:, :, 1], in_=oh2[:], axis=AX.X, op=ALU.add),
                     reads=["oh2"], writes=["slotf1"])
                S.op("dve", lambda: nc.vector.tensor_copy(out=sloti[:], in_=slotf[:]),
                     reads=["slotf0", "slotf1"], writes=["sloti"])
                for t in range(NT):
                    for k in range(2):
                        key = ("xgd", t, k)
                        xgd_keys.append(key)
                        S.dma("pool", lambda k=k: nc.gpsimd.indirect_dma_start(
                            out=self.xg_d[:, :], out_offset=bass.IndirectOffsetOnAxis(ap=sloti[:, t, k:k + 1], axis=0),
                            in_=self.x[:, t, :], in_offset=None),
                            reads=[("x", t), "sloti"] + [("xgd_init", q) for q in range(4)], writes=[key])
                    self.scale_x(t)
                S.barrier()

            xg = [self.sb(es, f"m_xg{k}", [128, D], BF16) for k in range(NBX)]
            xgT = [self.sb(es, f"m_xgT{k}", [128, 8, 128], BF16) for k in range(2)]
            hs = [self.sb(es, f"m_hs{k}", [128, 256], F32) for k in range(2)]
            hb = [self.sb(es, f"m_hb{k}", [128, 256], BF16) for k in range(2)]
            hT = [self.sb(es, f"m_hT{k}", [128, 2, 128], BF16) for k in range(2)]
            ysb = [self.sb(es, f"m_ysb{k}", [128, D], BF16) for k in range(2)]

            yd_keys = []

            def st_xg(e):
                bx = e % NBX
                S.dma("sp", lambda: nc.sync.dma_start(out=xg[bx][:], in_=self.xg_d[e * CAP:(e + 1) * CAP, :]),
                      reads=xgd_keys + [("xgd_init", q) for q in range(4)], writes=[("xg", bx)])

            def st_A(e):
                bx = e % NBX
                p2 = e % 2
                S.op("pe", [lambda c=c: nc.tensor.transpose(out=self.psB[:, p2, c * 128:(c + 1) * 128],
                                                            in_=xg[bx][:, c * 128:(c + 1) * 128], identity=self.ident_b[:])
                            for c in range(8)], reads=[("xg", bx), "ident_b"], writes=[("ps", p2)])
                S.op("act", lambda: nc.scalar.copy(out=xgT[p2][:], in_=self.psB[:, p2, :].rearrange("p (c n) -> p c n", c=8)),
                     reads=[("ps", p2)], writes=[("xgT", p2)])

            def st_B(e):
                b = e % NB
                p2 = e % 2
                S.op("pe", [lambda c=c: nc.tensor.matmul(self.psA[:, 2 + p2, :], lhsT=xgT[p2][:, c, :], rhs=W13[b][:, c, :],
                                                         start=(c == 0), stop=(c == 7)) for c in range(8)],
                     reads=[("xgT", p2), ("W13a", b), ("W13b", b)], writes=[("ps", 2 + p2)])
                S.op("act", lambda: nc.scalar.activation(out=hs[p2][:], in_=self.psA[:, 2 + p2, 0:256], func=AF.Silu),
                     reads=[("ps", 2 + p2)], writes=[("hs", p2)])
                S.op("dve", lambda: nc.vector.tensor_tensor(out=hb[p2][:], in0=self.psA[:, 2 + p2, 256:512], in1=hs[p2][:],
                                                            op=ALU.mult), reads=[("ps", 2 + p2), ("hs", p2)], writes=[("hb", p2)])
                if e + NB < 64:
                    load_w13(e + NB)

            def st_C(e):
                p2 = e % 2
                S.op("pe", [lambda q=q: nc.tensor.transpose(out=self.psB[:, 4, q * 128:(q + 1) * 128],
                                                            in_=hb[p2][:, q * 128:(q + 1) * 128], identity=self.ident_b[:])
                            for q in range(2)], reads=[("hb", p2), "ident_b"], writes=[("ps", 4)])
                S.op("act", lambda: nc.scalar.copy(out=hT[p2][:], in_=self.psB[:, 4, 0:256].rearrange("p (c n) -> p c n", c=2)),
                     reads=[("ps", 4)], writes=[("hT", p2)])

            def st_D(e):
                b = e % NB
                p2 = e % 2
                by = 5
                S.op("pe", [lambda q=q, hf=hf: nc.tensor.matmul(self.psA[:, by + hf, :], lhsT=hT[p2][:, q, :],
                                                                rhs=W2[b][:, q, hf * 512:(hf + 1) * 512],
                                                                start=(q == 0), stop=(q == 1))
                            for hf in range(2) for q in range(2)],
                     reads=[("hT", p2), ("W2", b)], writes=[("ps", by), ("ps", by + 1)])
                S.op("dve", lambda: nc.vector.tensor_copy(out=ysb[p2][:], in_=self.psA[:, by:by + 2, :].rearrange("p a n -> p (a n)")),
                     reads=[("ps", by), ("ps", by + 1)], writes=[("ysb", p2)])
                key = ("yd", e)
                yd_keys.append(key)
                S.dma("sp", lambda: nc.sync.dma_start(out=self.y_d[e * CAP:(e + 1) * CAP, :], in_=ysb[p2][:]),
                      reads=[("ysb", p2)], writes=[key])
                if e + NB < 64:
                    load_w2(e + NB)

            st_xg(0)
            st_xg(1)
            for s_ in range(64 + 3):
                if s_ + 2 < 64:
                    st_xg(s_ + 2)
                if s_ < 64:
                    st_A(s_)
                if 0 <= s_ - 1 < 64:
                    st_B(s_ - 1)
                if 0 <= s_ - 2 < 64:
                    st_C(s_ - 2)
                if 0 <= s_ - 3 < 64:
                    st_D(s_ - 3)

            NG = 8
            gbuf = [W13[k // 4][:, (k % 4) * 2:(k % 4) * 2 + 2, :].rearrange("p a n -> p (a n)") for k in range(NG)]
            wkeys = [("W13a", 0), ("W13b", 0), ("W13a", 1), ("W13b", 1)]

            def gather(t):
                for k in range(2):
                    gi = (2 * t + k) % NG
                    S.dma("pool", lambda k=k, gi=gi: nc.gpsimd.indirect_dma_start(
                        out=gbuf[gi], out_offset=None, in_=self.y_d[:, :],
                        in_offset=bass.IndirectOffsetOnAxis(ap=sloti[:, t, k:k + 1], axis=0)),
                        reads=yd_keys + ["sloti"], writes=[("gb", gi)])

            S.acquire("pool", wkeys)
            for t in range(4):
                gather(t)
            for t0 in range(0, NT, 2):
                for t in (t0, t0 + 1):
                    for k in range(2):
                        gi = (2 * t + k) % NG
                        S.op("dve", lambda t=t, k=k, gi=gi: nc.vector.scalar_tensor_tensor(
                            out=self.x[:, t, :], in0=gbuf[gi], scalar=gates[:, t, k:k + 1], in1=self.x[:, t, :],
                            op0=ALU.mult, op1=ALU.add),
                            reads=[("gb", gi), "gates0", "gates1", ("x", t)], writes=[("x", t)])
                for t in (t0 + 4, t0 + 5):
                    if t < NT:
                        gather(t)
                self.layer_norm_tiles([t0, t0 + 1])
                if make_xT_after:
                    self.make_xT(t0)
                    self.make_xT(t0 + 1, banks=(4, 5))
            S.barrier()


def prep_inputs(inputs):
    shared = {}
    for n in ("moba_w_qkv", "moba_w_o", "gmlp_w_in", "gmlp_b_in", "gmlp_ln_g", "gmlp_ln_b", "gmlp_w_out",
              "ln1_g", "ln1_b", "ln2_g", "ln2_b", "moe_w1", "moe_w3", "moe_w2"):
        shared[n] = np.ascontiguousarray(inputs[n], dtype=np.float32)
    shared["gmlp_w_sT"] = np.ascontiguousarray(np.transpose(inputs["gmlp_w_s"], (0, 3, 1, 2)))
    shared["gmlp_b_sT"] = np.ascontiguousarray(np.transpose(inputs["gmlp_b_s"], (0, 2, 1)))
    shared["moe_w_r"] = np.ascontiguousarray(np.concatenate([inputs["moe_w_grp"], inputs["moe_w_rt"]], axis=2))
    shared["moe_b_r"] = np.ascontiguousarray(np.concatenate([inputs["moe_b_grp"], inputs["moe_b_rt"]], axis=1))
    shared.update(host_consts())
    return shared


FULL_STAGES = [("moba", 0), ("moe", 0), ("gmlp", 1), ("moe", 1), ("moba", 2), ("moe", 2), ("gmlp", 3), ("moe", 3)]


def run(inputs, stages, n_cores=8, trace=False, strict_same=True):
    shared = prep_inputs(inputs)
    kb = K(stages, strict_same=strict_same)
    nc = kb.build()
    x = np.ascontiguousarray(inputs["x"], dtype=np.float32)
    in_maps = []
    for c in range(n_cores):
        m = dict(shared)
        m["x"] = x[c]
        in_maps.append(m)
    res = run_bass_kernel_spmd(nc, in_maps, core_ids=list(range(n_cores)), trace=trace)
    out = np.stack([r["out"] for r in res.results], axis=0)
    return out, res


def kernel(**inputs):
    out, _ = run(inputs, FULL_STAGES, n_cores=8)
    return out.astype(np.float32)
```
